# Optimizing a Trainium2 kernel written in Bass

```python
import math
import jax, jax.numpy as jnp
from jax import lax
import numpy as np

D_MODEL = 1024
BATCH = 4
SEQ = 4096
DEPTH = 1

HEAD_DIM = 64
N_FOX_HEADS = 8
N_DSA_HEADS = 8
N_IDX_HEADS = 4
IDX_DIM = 64
TOPK_MAX = 256
Q_BLOCK = 128
N_BUCKETS = 32
MAX_DISTANCE = 128
D_FF = 4 * D_MODEL
EPS = 1e-6

FOX_W = N_FOX_HEADS * HEAD_DIM
DSA_W = N_DSA_HEADS * HEAD_DIM
SPLIT_SIZES = [
    FOX_W, FOX_W, FOX_W,
    N_FOX_HEADS,
    DSA_W, DSA_W, DSA_W,
    N_IDX_HEADS * IDX_DIM,
    IDX_DIM,
    N_IDX_HEADS,
    D_MODEL, D_MODEL,
]
D_IN = int(sum(SPLIT_SIZES))
SPLIT_POINTS = [int(v) for v in np.cumsum(SPLIT_SIZES)[:-1]]

kernel_name = "hybrid_fox_dsa_gated_block"


def rmsnorm(x, g):
    xf = x.astype(jnp.float32)
    y = xf * lax.rsqrt(jnp.mean(xf * xf, axis=-1, keepdims=True) + EPS)
    return (y * g.astype(jnp.float32)).astype(x.dtype)


def t5_bucket(dist):
    n = jnp.maximum(dist, 0)
    max_exact = N_BUCKETS // 2
    is_small = n < max_exact
    nf = jnp.maximum(n, 1).astype(jnp.float32)
    large = max_exact + (jnp.log(nf / max_exact) / math.log(MAX_DISTANCE / max_exact)
                         * (N_BUCKETS - max_exact)).astype(jnp.int32)
    large = jnp.minimum(large, N_BUCKETS - 1)
    return jnp.where(is_small, n, large)


def forgetting_attention(q, k, v, cum_logf):
    B, S, H, Dh = q.shape
    n_blocks = S // Q_BLOCK
    scale = Dh ** -0.5
    Ft = jnp.transpose(cum_logf, (0, 2, 1))
    key_pos = jnp.arange(S)

    def block(i):
        start = i * Q_BLOCK
        qb = lax.dynamic_slice_in_dim(q, start, Q_BLOCK, axis=1)
        Fb = lax.dynamic_slice_in_dim(Ft, start, Q_BLOCK, axis=2)
        s = jnp.einsum('bqhd,bkhd->bhqk', qb, k).astype(jnp.float32) * scale
        s = s + Fb[..., :, None] - Ft[:, :, None, :]
        q_pos = start + jnp.arange(Q_BLOCK)
        mask = key_pos[None, :] <= q_pos[:, None]
        s = jnp.where(mask[None, None], s, -jnp.inf)
        p = jax.nn.softmax(s, axis=-1).astype(v.dtype)
        return jnp.einsum('bhqk,bkhd->bqhd', p, v)

    out = lax.map(block, jnp.arange(n_blocks))
    return jnp.transpose(out, (1, 0, 2, 3, 4)).reshape(B, S, H, Dh)


def indexer_sparse_attention(q, k, v, q_idx, k_idx, w_idx, rel_bias):
    B, S, H, Dh = q.shape
    n_blocks = S // Q_BLOCK
    topk = min(TOPK_MAX, S // 4)
    scale = Dh ** -0.5
    idx_scale = IDX_DIM ** -0.5
    key_pos = jnp.arange(S)
    gather = jax.vmap(lambda arr, ids: arr[ids])

    def block(i):
        start = i * Q_BLOCK
        qb = lax.dynamic_slice_in_dim(q, start, Q_BLOCK, axis=1)
        qib = lax.dynamic_slice_in_dim(q_idx, start, Q_BLOCK, axis=1)
        wib = lax.dynamic_slice_in_dim(w_idx, start, Q_BLOCK, axis=1)
        q_pos = start + jnp.arange(Q_BLOCK)
        sc = jnp.einsum('bqhd,bkd->bqhk', qib, k_idx).astype(jnp.float32) * idx_scale
        iscore = jnp.einsum('bqhk,bqh->bqk', jax.nn.relu(sc), wib.astype(jnp.float32))
        causal = key_pos[None, :] <= q_pos[:, None]
        iscore = jnp.where(causal[None], iscore, -jnp.inf)
        _, sel = lax.top_k(iscore, topk)
        kg = gather(k, sel)
        vg = gather(v, sel)
        logits = jnp.einsum('bqhd,bqkhd->bhqk', qb, kg).astype(jnp.float32) * scale
        dist = q_pos[None, :, None] - sel
        valid = dist >= 0
        bias = rel_bias[t5_bucket(dist)].astype(jnp.float32)
        logits = logits + jnp.transpose(bias, (0, 3, 1, 2))
        logits = jnp.where(valid[:, None], logits, -jnp.inf)
        p = jax.nn.softmax(logits, axis=-1).astype(vg.dtype)
        return jnp.einsum('bhqk,bqkhd->bqhd', p, vg)

    out = lax.map(block, jnp.arange(n_blocks))
    return jnp.transpose(out, (1, 0, 2, 3, 4)).reshape(B, S, H, Dh)


def setup_inputs(seed: int = 0) -> dict:
    key = jax.random.key(seed)
    ks = jax.random.split(key, 16)
    D = D_MODEL
    nrm = lambda k, shape, s: jax.random.normal(k, shape, jnp.float32) * s
    return {
        "x": nrm(ks[0], (BATCH, SEQ, D), 1.0),
        "c": nrm(ks[1], (BATCH, D), 1.0),
        "w_ada": nrm(ks[2], (D, 6 * D), 0.5 * D ** -0.5),
        "b_ada": nrm(ks[3], (6 * D,), 0.02),
        "g_norm1": 1.0 + nrm(ks[4], (D,), 0.02),
        "w_in": nrm(ks[5], (D, D_IN), D ** -0.5),
        "b_forget": nrm(ks[6], (N_FOX_HEADS,), 0.1),
        "rel_bias": nrm(ks[7], (N_BUCKETS, N_DSA_HEADS), 0.5),
        "w_branch_fox": nrm(ks[8], (FOX_W, D), FOX_W ** -0.5),
        "w_branch_dsa": nrm(ks[9], (DSA_W, D), DSA_W ** -0.5),
        "w_out": nrm(ks[10], (D, D), D ** -0.5),
        "g_norm2": 1.0 + nrm(ks[11], (D,), 0.02),
        "w_mlp1": nrm(ks[12], (D, D_FF), D ** -0.5),
        "w_mlp2": nrm(ks[13], (D_FF, D), D_FF ** -0.5),
        "g_final": 1.0 + nrm(ks[14], (D,), 0.02),
    }


def reference(x, c, w_ada, b_ada, g_norm1, w_in, b_forget, rel_bias, w_branch_fox,
              w_branch_dsa, w_out, g_norm2, w_mlp1, w_mlp2, g_final):
    B, S, D = x.shape
    ada = (c @ w_ada + b_ada)[:, None, :]
    shift1, scale1, gate1, shift2, scale2, gate2 = jnp.split(ada, 6, axis=-1)

    for _ in range(DEPTH):
        h = rmsnorm(x, g_norm1) * (1.0 + scale1) + shift1
        proj = h @ w_in
        (q_f, k_f, v_f, f_logit, q_d, k_d, v_d, q_i, k_i, w_i,
         gate_fox, gate_dsa) = jnp.split(proj, SPLIT_POINTS, axis=-1)
        hs = (B, S, -1, HEAD_DIM)
        log_f = jax.nn.log_sigmoid((f_logit + b_forget).astype(jnp.float32))
        cum_logf = jnp.cumsum(log_f, axis=1)
        y_fox = forgetting_attention(q_f.reshape(hs), k_f.reshape(hs), v_f.reshape(hs),
                                     cum_logf).reshape(B, S, FOX_W)
        w_i = w_i * (N_IDX_HEADS ** -0.5)
        y_dsa = indexer_sparse_attention(
            q_d.reshape(hs), k_d.reshape(hs), v_d.reshape(hs),
            q_i.reshape(B, S, N_IDX_HEADS, IDX_DIM), k_i, w_i, rel_bias
        ).reshape(B, S, DSA_W)
        merged = (jax.nn.sigmoid(gate_fox) * (y_fox @ w_branch_fox)
                  + jax.nn.sigmoid(gate_dsa) * (y_dsa @ w_branch_dsa))
        x = x + gate1 * (merged @ w_out)

        h2 = rmsnorm(x, g_norm2) * (1.0 + scale2) + shift2
        x = x + gate2 * (jnp.square(jax.nn.relu(h2 @ w_mlp1)) @ w_mlp2)

    return rmsnorm(x, g_final)
```

```python
import math
from contextlib import ExitStack
import numpy as np
import concourse.bass as bass
import concourse.mybir as mybir
from concourse.bass_utils import run_bass_kernel_spmd

F32 = mybir.dt.float32
BF16 = mybir.dt.bfloat16
ALU = mybir.AluOpType
AF = mybir.ActivationFunctionType
AX = mybir.AxisListType

D = 1024
S_LEN = 4096
NT = 32
NSLOT = 16
HD = 64
NBIS = 12
NEG = -30000.0
_DBG_DSLOTS = NSLOT
QF0, KF0, VF0, FL0, QD0, KD0, VD0, QI0, KI0, WI0, GF0, GD0 = (
    0, 512, 1024, 1536, 1544, 2056, 2568, 3080, 3336, 3400, 3404, 4428)
PV_C, PV_BADA, PV_G1, PV_G2, PV_GF = 0, 8, 56, 64, 72
PV_PAR0, PV_PAR1, PV_PAR0S, PV_PAR1S = 80, 81, 82, 83
PV_BF = 84
PV_SK = 86
PV_SQ = 90
PV_B31 = 102
PV_EPS = 110
NPV = 112


class Buf:
    __slots__ = ("w", "r")

    def __init__(self):
        self.w = None
        self.r = []


class Sched:
    ENGS = ("pe", "act", "dve", "pool", "sp")

    def __init__(self, nc, n_dma_sems=32):
        self.nc = nc
        self.h = {"pe": nc.tensor, "act": nc.scalar, "dve": nc.vector,
                  "pool": nc.gpsimd, "sp": nc.sync}
        self.sem = {e: nc.alloc_semaphore("c_" + e) for e in ("pe", "act", "dve", "pool")}
        self.cnt = {e: 0 for e in self.sem}
        self.known = {e: {} for e in self.ENGS}
        self.dsem = {q: [nc.alloc_semaphore("d%s%d" % (q, i)) for i in range(n_dma_sems)] for q in ("sp", "pool")}
        self.dcnt = {q: [0] * n_dma_sems for q in ("sp", "pool")}
        self.dnext = {"sp": 0, "pool": 0}

    def _semh(self, key):
        return self.sem[key] if isinstance(key, str) else self.dsem[key[1]][key[2]]

    def wait(self, eng, tickets):
        best = {}
        for t in tickets:
            if t is None:
                continue
            k, v = t
            if k == eng and eng == "pe":
                continue
            if best.get(k, 0) < v:
                best[k] = v
        kn = self.known[eng]
        for k, v in best.items():
            if kn.get(k, 0) >= v:
                continue
            self.h[eng].wait_ge(self._semh(k), v)
            kn[k] = v

    def _deps(self, reads, writes):
        deps = []
        for b in reads:
            deps.append(b.w)
        for b in writes:
            deps.append(b.w)
            deps.extend(b.r)
        return deps

    def _commit(self, t, reads, writes):
        for b in reads:
            b.r.append(t)
        for b in writes:
            b.w = t
            b.r = []

    def op(self, eng, fn, reads=(), writes=()):
        self.wait(eng, self._deps(reads, writes))
        ins = fn(self.h[eng])
        self.cnt[eng] += 1
        ins.then_inc(self.sem[eng], 1)
        t = (eng, self.cnt[eng])
        self._commit(t, reads, writes)
        return t

    def dma(self, out, in_, reads=(), writes=(), q="sp"):
        deps = self._deps(reads, writes)
        i = self.dnext[q]
        self.dnext[q] = (i + 1) % len(self.dsem[q])
        if self.dcnt[q][i] > 0:
            deps.append((("d", q, i), self.dcnt[q][i]))
        self.wait(q, deps)
        self.dcnt[q][i] += 16
        self.h[q].dma_start(out=out, in_=in_).then_inc(self.dsem[q][i], 16)
        t = (("d", q, i), self.dcnt[q][i])
        self._commit(t, reads, writes)
        return t


def _barrier(self):
    tickets = [(e, c) for e, c in self.cnt.items() if c > 0]
    tickets += [(("d", q, i), v) for q in self.dcnt for i, v in enumerate(self.dcnt[q]) if v > 0]
    for e in self.ENGS:
        self.wait(e, tickets)


Sched.barrier = _barrier


class TB:
    def __init__(self, t, n=1):
        self.t = t
        self.b = Buf()
        self.bs = [Buf() for _ in range(n)]


class _Stop(Exception):
    pass


def build_program(stop=None):
    holder = {}
    try:
        _build(holder, stop)
    except _Stop:
        pass
    holder["ES"].close()
    return holder["nc"]


def _build(holder, stop):
    stop_sigma = 1
    if stop and "@" in stop:
        stop, ss = stop.split("@")
        stop_sigma = int(ss)
    nc = bass.Bass("TRN2", target_bir_lowering=False)
    holder["nc"] = nc
    dt = nc.dram_tensor
    xall = dt("xall", [S_LEN, D], F32, kind="ExternalInput").ap()
    xown = dt("xown", [NSLOT * 128, D], F32, kind="ExternalInput").ap()
    pvd = dt("pv", [128, NPV], F32, kind="ExternalInput").ap()
    w_ada = dt("w_ada", [D, 6 * D], F32, kind="ExternalInput").ap()
    w_in = dt("w_in", [D, 5452], F32, kind="ExternalInput").ap()
    wfpad = dt("wfpad", [D, 128], F32, kind="ExternalInput").ap()
    wkidup = dt("wkidup", [D, 128], F32, kind="ExternalInput").ap()
    identd = dt("ident", [128, 128], F32, kind="ExternalInput").ap()
    maskFd = dt("maskF", [128, 4 * 128], F32, kind="ExternalInput").ap()
    maskId = dt("maskI", [128, 2 * 256], F32, kind="ExternalInput").ap()
    bmatd = dt("bmat", [128, 8 * 6 * 128], F32, kind="ExternalInput").ap()
    wbfd = dt("w_branch_fox", [512, D], F32, kind="ExternalInput").ap()
    wbdd = dt("w_branch_dsa", [512, D], F32, kind="ExternalInput").ap()
    w_out = dt("w_out", [D, D], F32, kind="ExternalInput").ap()
    w_mlp1 = dt("w_mlp1", [D, 4 * D], F32, kind="ExternalInput").ap()
    w_mlp2 = dt("w_mlp2", [4 * D, D], F32, kind="ExternalInput").ap()
    outd = dt("out", [NSLOT * 128, D], F32, kind="ExternalOutput").ap()
    wgb = dt("wgb", [D, 2048], BF16, kind="Internal").ap()
    wbfb = dt("wbfb", [512, D], BF16, kind="Internal").ap()
    wbdb = dt("wbdb", [512, D], BF16, kind="Internal").ap()
    wob = dt("wob", [D, D], BF16, kind="Internal").ap()
    w1b = dt("w1b", [D, 4 * D], BF16, kind="Internal").ap()
    w2b = dt("w2b", [4 * D, D], BF16, kind="Internal").ap()

    S = Sched(nc)
    ES = ExitStack()
    convb = Buf()
    holder["ES"] = ES
    holder["S"] = S

    uid = {"i": 0}

    def sb(es, name, shape, dtype, n=1):
        uid["i"] += 1
        return TB(es.enter_context(nc.sbuf_tensor("s%d_%s" % (uid["i"], name), shape, dtype)), n)

    def MM(out, lhsT, rhs, start, stop, r, w):
        return S.op("pe", lambda e: e.matmul(out, lhsT=lhsT, rhs=rhs, start=start, stop=stop), r, w)

    def TR(out, in_, ident, r, w):
        return S.op("pe", lambda e: e.transpose(out=out, in_=in_, identity=ident), r, w)

    def ACTV(out, in_, func, r, w, **kw):
        return S.op("act", lambda e: e.activation(out=out, in_=in_, func=func, **kw), r, w)

    def TS(eng, out, in0, s1, s2, op0, op1, r, w, **kw):
        if op1 is None:
            return S.op(eng, lambda e: e.tensor_scalar(out=out, in0=in0, scalar1=s1, scalar2=None, op0=op0, **kw), r, w)
        return S.op(eng, lambda e: e.tensor_scalar(out=out, in0=in0, scalar1=s1, scalar2=s2, op0=op0, op1=op1, **kw), r, w)

    def STT(out, in0, scalar, in1, op0, op1, r, w):
        return S.op("dve", lambda e: e.scalar_tensor_tensor(out=out, in0=in0, scalar=scalar, in1=in1, op0=op0, op1=op1), r, w)

    def TT(eng, out, in0, in1, op, r, w):
        return S.op(eng, lambda e: e.tensor_tensor(out=out, in0=in0, in1=in1, op=op), r, w)

    def CP(eng, out, in_, r, w):
        if eng == "act":
            return ACTV(out, in_, AF.Copy, r, w)
        return S.op(eng, lambda e: e.tensor_copy(out=out, in_=in_), r, w)

    def MSET(eng, ap, val, w):
        return S.op(eng, lambda e: e.memset(ap, val), (), w)

    Stop = _Stop

    def dump(items):
        S.barrier()
        with ExitStack() as ed:
            stg = sb(ed, "dbgstg", [128, 1024], F32)
            c0 = 0
            for ap, bufs, ncols, isf in items:
                CP("dve", stg.t[:, c0:c0 + ncols], ap, bufs, [stg.b])
                c0 += ncols
            t = S.dma(outd[0:128, 0:c0], stg.t[:, 0:c0], reads=[stg.b])
            S.wait("sp", [t])

    psb = [TB(ES.enter_context(nc.psum_tensor("ps%d" % i, [128, 512], F32))) for i in range(6)]
    psTs = [TB(ES.enter_context(nc.psum_tensor("psT%d" % i, [128, 1024], BF16))) for i in range(2)]
    ring = {"i": 0}
    OB0, SSB = 3, 5

    def gbank():
        k = ring["i"]
        ring["i"] = (k + 1) % 3
        return psb[k]

    pv = sb(ES, "pv", [128, NPV], F32)
    identf = sb(ES, "identf", [128, 128], F32)
    identb = sb(ES, "identb", [128, 128], BF16)
    onesf = sb(ES, "onesf", [128, 512], F32)
    adaT = sb(ES, "adaT", [128, 48], F32)
    A12 = sb(ES, "A12", [128, 16], F32)
    yfoxT = sb(ES, "yfoxT", [128, 4, NSLOT * 128], BF16)
    ydsaT = sb(ES, "ydsaT", [128, 4, NSLOT * 128], BF16)

    S.dma(pv.t[:], pvd[:, :], writes=[pv.b])
    S.dma(identf.t[:], identd[:, :], writes=[identf.b])
    CP("dve", identb.t[:], identf.t[:], [identf.b], [identb.b])
    MSET("dve", onesf.t[:], 1.0, [onesf.b])

    def pvc(c, n=1):
        return pv.t[:, c:c + n]

    with ExitStack() as es:
        wa = sb(es, "wa", [128, 4, 8, 512], F32, 4)
        acc = psb[SSB]
        for cbk in range(12):
            sl = cbk % 4
            S.dma(wa.t[:, sl, :, :], w_ada[:, cbk * 512:(cbk + 1) * 512].rearrange("(c p) n -> p c n", p=128),
                  writes=[wa.bs[sl]])
            for j4 in range(4):
                j = cbk * 4 + j4
                for kc in range(8):
                    MM(acc.t[:, j:j + 1], wa.t[:, sl, kc, j4 * 128:(j4 + 1) * 128], pv.t[:, PV_C + kc:PV_C + kc + 1],
                       kc == 0, kc == 7, [wa.bs[sl], pv.b], [acc.b])
        TT("dve", adaT.t[:, :], acc.t[:, 0:48], pv.t[:, PV_BADA:PV_BADA + 48], ALU.add, [acc.b, pv.b], [adaT.b])
        STT(A12.t[:, 0:8], adaT.t[:, 8:16], 1.0, pv.t[:, PV_G1:PV_G1 + 8], ALU.add, ALU.mult, [adaT.b, pv.b], [A12.b])
        STT(A12.t[:, 8:16], adaT.t[:, 32:40], 1.0, pv.t[:, PV_G2:PV_G2 + 8], ALU.add, ALU.mult, [adaT.b, pv.b], [A12.b])
    S.barrier()
    if stop == "A":
        dump([(adaT.t[:, :], [adaT.b], 48, True), (A12.t[:, :], [A12.b], 16, True)])
        return
    SH1 = lambda c: adaT.t[:, 0 + c:1 + c]
    G1 = lambda c: adaT.t[:, 16 + c:17 + c]
    SH2 = lambda c: adaT.t[:, 24 + c:25 + c]
    G2 = lambda c: adaT.t[:, 40 + c:41 + c]
    A1 = lambda c: A12.t[:, c:c + 1]
    A2 = lambda c: A12.t[:, 8 + c:9 + c]
    cb = [adaT.b, A12.b, pv.b]

    def stats_rstd(es_tiles, src_chunks, T, rstd):
        sq = es_tiles["sq"]
        ssb = psb[SSB]
        pend = None
        for c in range(8):
            ap, bufs = src_chunks[c]
            sl = c % 2
            ACTV(sq.t[:, sl, :T], ap, AF.Square, bufs, [sq.bs[sl]])
            if pend is not None:
                pend()
            pend = (lambda c=c, sl=sl: MM(ssb.t[:, :T], onesf.t[:, 0:128], sq.t[:, sl, :T], c == 0, c == 7,
                                          [onesf.b, sq.bs[sl]], [ssb.b]))
        pend()
        ACTV(rstd.t[:, :T], ssb.t[:, :T], AF.Ln, [ssb.b, pv.b], [rstd.b], scale=1.0 / D, bias=pvc(PV_EPS))
        ACTV(rstd.t[:, :T], rstd.t[:, :T], AF.Exp, [rstd.b], [rstd.b], scale=-0.5)

    def load_T(tl, src_tiles, T, keep_xT):
        xt, xT = tl["xt"], tl["xT"]
        ntt = T // 128
        for tt in range(ntt):
            S.dma(xt.t[:, tt, :], src_tiles[tt], writes=[xt.bs[tt]])
        for c in range(8):
            bk = gbank()
            for tt in range(ntt):
                TR(bk.t[:, tt * 128:(tt + 1) * 128], xt.t[:, tt, c * 128:(c + 1) * 128], identf.t[:, :],
                   [xt.bs[tt], identf.b], [bk.b])
            CP("dve", xT.t[:, c, :T], bk.t[:, :T], [bk.b], [xT.bs[c]])
        return [(xT.t[:, c, :T], [xT.bs[c]]) for c in range(8)]

    def modulate(tl, T, rstd, Af, SHf, hT):
        xT, tmp = tl["xT"], tl["tmp"]
        for c in range(8):
            sl = c % 2
            STT(tmp.t[:, sl, :T], xT.t[:, c, :T], Af(c), rstd.t[:, :T], ALU.mult, ALU.mult,
                [xT.bs[c], rstd.b] + cb, [tmp.bs[sl]])
            ACTV(hT.t[:, c, :T], tmp.t[:, sl, :T], AF.Identity, [tmp.bs[sl]] + cb, [hT.bs[c]],
                 bias=SHf(c), scale=1.0)

    def load_w(dst_ap, src_ap, w):
        return S.dma(dst_ap, src_ap.rearrange("(c p) n -> p c n", p=128), writes=w, q="pool")

    def projT(w_ap_fn, wbuf, hT, T, M):
        bk = gbank()
        for c in range(8):
            MM(bk.t[:M, :T], w_ap_fn(c), hT.t[:, c, :T], c == 0, c == 7, [hT.bs[c], wbuf], [bk.b])
        return bk

    def par_sel(blk):
        return (PV_PAR0, PV_PAR1) if blk % 2 == 0 else (PV_PAR1, PV_PAR0)

    def attn_finish(Y, yT, sigma):
        psT = psTs[sigma % 2]
        for fc in range(4):
            TR(psT.t[:, fc * 128:(fc + 1) * 128], Y.t[:, fc * 128:(fc + 1) * 128],
               identb.t[:, :], [Y.b, identb.b], [psT.b])
        CP("act", yT.t[:, :, sigma * 128:(sigma + 1) * 128],
           psT.t[:, 0:512].rearrange("p (f q) -> p f q", f=4), [psT.b], [yT.b])

    def pv_norm(Ob, Y, h, rec):
        S.op("dve", lambda e: e.reciprocal(out=rec.t[:, 0:1], in_=Ob.t[:, 64:65]), [Ob.b], [rec.b])
        TS("dve", Y.t[:, h * 64:(h + 1) * 64], Ob.t[:, 0:64], rec.t[:, 0:1], None, ALU.mult, None,
           [Ob.b, rec.b], [Y.b])

    def pv_norm_act(Ob, Y, h, rec):
        ACTV(rec.t[:, 0:1], Ob.t[:, 64:65], AF.Ln, [Ob.b], [rec.b])
        ACTV(rec.t[:, 0:1], rec.t[:, 0:1], AF.Exp, [rec.b], [rec.b], scale=-1.0)
        ACTV(Y.t[:, h * 64:(h + 1) * 64], Ob.t[:, 0:64], AF.Identity, [Ob.b, rec.b], [Y.b], scale=rec.t[:, 0:1])

    def run_attention(ntile, front, back, norm, finish, norm_delay, look=1):
        jobs = [(h, jb) for h in range(8) for jb in range(0, ntile, 4)]
        ctx = {}
        for i in range(min(look, len(jobs))):
            ctx[i] = front(*jobs[i])
        pending = []
        for i, (h, jb) in enumerate(jobs):
            if i + look < len(jobs):
                ctx[i + look] = front(*jobs[i + look])
            back(h, jb, ctx.pop(i))
            if jb + 4 >= ntile:
                pending.append((i + norm_delay, h))
            while pending and pending[0][0] <= i:
                norm(pending.pop(0)[1])
        for _, h in pending:
            norm(h)
        finish()

    with ExitStack() as es:
        KF = sb(es, "KF", [128, 4, S_LEN], BF16, 16)
        KA = sb(es, "KA", [128, S_LEN], BF16, 16)
        VF = sb(es, "VF", [128, NT, 8 * 65], BF16, 16)
        QF = sb(es, "QF", [128, 4, NSLOT * 128], BF16, 16)
        QA = [sb(es, "QA%d" % v, [128, NSLOT * 128], BF16, 16) for v in range(3)]
        maskFb = sb(es, "maskFb", [128, 4 * 128], BF16)
        S.dma(maskFb.t[:], maskFd[:, :], writes=[maskFb.b], q="pool")
        MSET("pool", VF.t[:, :, :].rearrange("p t (h e) -> p t h e", h=8)[:, :, :, 64:65], 1.0, VF.bs)
        with ExitStack() as eb:
            wF = sb(eb, "wF", [128, 8, 1536 + 128], BF16)
            load_w(wF.t[:, :, 0:1536], w_in[:, 0:1536], [wF.b])
            load_w(wF.t[:, :, 1536:1664], wfpad[:, :], [wF.b])
            for dst, src, step in ((wgb, w_in[:, GF0:GF0 + 2048], 512), (wbfb, wbfd, 512), (wbdb, wbdd, 512),
                                   (wob, w_out, 1024), (w1b, w_mlp1, 256), (w2b, w_mlp2, 1024)):
                for r0 in range(0, dst.shape[0], step):
                    S.dma(dst[r0:r0 + step, :], src[r0:r0 + step, :], writes=[convb], q="pool")
            tl = {"xt": sb(eb, "xt", [128, 2, D], F32, 2), "xT": sb(eb, "xT", [128, 8, 256], F32, 8),
                  "sq": sb(eb, "sq", [128, 2, 256], F32, 2), "tmp": sb(eb, "tmp", [128, 2, 256], F32, 2)}
            tl2 = dict(tl)
            tl2["xT"] = sb(eb, "xTb", [128, 8, 256], F32, 8)
            tls = [tl, tl2]
            rstd = sb(eb, "rstd", [128, 256], F32)
            hTs = [sb(eb, "hT%d" % i, [128, 8, 256], BF16, 8) for i in range(2)]
            sc = [sb(eb, "scn%d" % i, [128, 256], F32) for i in range(4)]
            lv = [sb(eb, "lv%d" % i, [128, 256], BF16) for i in range(3)]
            carry = sb(eb, "carry", [128, 1], F32)
            qtmp = sb(eb, "qtmp", [128, 128], F32)
            MSET("dve", carry.t[:, :], 0.0, [carry.b])
            T = 256
            for blk in range(16):
                hT = hTs[blk % 2]
                tl = tls[blk % 2]
                src = load_T(tl, [xall[blk * 256 + tt * 128: blk * 256 + (tt + 1) * 128, :] for tt in range(2)], T, False)
                stats_rstd(tl, src, T, rstd)
                modulate(tl, T, rstd, A1, SH1, hT)
                sig = blk
                selA, selB = par_sel(blk)
                kb = blk // 1
                for pr in range(4):
                    bk = projT(lambda c, pr=pr: wF.t[:, c, 512 + pr * 128: 512 + (pr + 1) * 128], wF.b, hT, T, 128)
                    CP("act", KF.t[:, pr, blk * 256:(blk + 1) * 256], bk.t[:, :T], [bk.b, wF.b], [KF.bs[kb]])
                for pr in range(4):
                    bk = projT(lambda c, pr=pr: wF.t[:, c, pr * 128:(pr + 1) * 128], wF.b, hT, T, 128)
                    TS("dve", qtmp.t[:, :], bk.t[:, 0:128], pvc(selA), None, ALU.mult, None, [bk.b, pv.b, wF.b], [qtmp.b])
                    STT(QF.t[:, pr, sig * 128:(sig + 1) * 128], bk.t[:, 128:256], pvc(selB), qtmp.t[:, :],
                        ALU.mult, ALU.add, [bk.b, qtmp.b, pv.b], [QF.bs[sig]])
                for tt in range(2):
                    bk = gbank()
                    for c in range(8):
                        MM(bk.t[:, :512], hT.t[:, c, tt * 128:(tt + 1) * 128], wF.t[:, c, 1024:1536], c == 0, c == 7,
                           [hT.bs[c], wF.b], [bk.b])
                    tile_i = blk * 2 + tt
                    CP("act", VF.t[:, tile_i, :].rearrange("p (h e) -> p h e", h=8)[:, :, 0:64],
                       bk.t[:, :512].rearrange("p (h d) -> p h d", h=8), [bk.b], [VF.bs[kb]])
                bk = projT(lambda c: wF.t[:, c, 1536:1664], wF.b, hT, T, 128)
                ACTV(sc[0].t[:, :], bk.t[:, :T], AF.Sigmoid, [bk.b, pv.b, wF.b], [sc[0].b], bias=pvc(PV_BF), scale=1.0)
                ACTV(sc[1].t[:, :], sc[0].t[:, :], AF.Ln, [sc[0].b], [sc[1].b])
                S.op("dve", lambda e: e.tensor_tensor_scan(out=sc[2].t[:, :], data0=onesf.t[:, 0:256], data1=sc[1].t[:, :],
                                                            initial=carry.t[:, 0:1], op0=ALU.mult, op1=ALU.add),
                     [sc[1].b, onesf.b, carry.b], [sc[2].b])
                CP("dve", carry.t[:, 0:1], sc[2].t[:, 255:256], [sc[2].b], [carry.b])
                TS("dve", lv[0].t[:, :], sc[2].t[:, :], -8.0, None, ALU.mult, None, [sc[2].b], [lv[0].b])
                STT(sc[3].t[:, :], sc[2].t[:, :], -8.0, lv[0].t[:, :], ALU.mult, ALU.subtract, [sc[2].b, lv[0].b], [sc[3].b])
                CP("dve", lv[1].t[:, :], sc[3].t[:, :], [sc[3].b], [lv[1].b])
                TT("dve", lv[2].t[:, :], sc[3].t[:, :], lv[1].t[:, :], ALU.subtract, [sc[3].b, lv[1].b], [lv[2].b])
                TS("dve", sc[0].t[:, :], lv[0].t[:, :], pvc(PV_SK + 0), None, ALU.mult, None, [lv[0].b, pv.b], [sc[0].b])
                STT(sc[0].t[:, :], lv[1].t[:, :], pvc(PV_SK + 1), sc[0].t[:, :], ALU.mult, ALU.add, [lv[1].b, sc[0].b], [sc[0].b])
                STT(sc[0].t[:, :], lv[2].t[:, :], pvc(PV_SK + 2), sc[0].t[:, :], ALU.mult, ALU.add, [lv[2].b, sc[0].b], [sc[0].b])
                TS("dve", KA.t[:, blk * 256:(blk + 1) * 256], sc[0].t[:, :], pvc(PV_SK + 3), None, ALU.add, None,
                   [sc[0].b, pv.b], [KA.bs[kb]])
                for v in range(3):
                    q0 = PV_SQ + 4 * v
                    TS("dve", sc[1].t[:, :], lv[0].t[:, :], pvc(q0 + 0), None, ALU.mult, None, [lv[0].b, pv.b], [sc[1].b])
                    STT(sc[1].t[:, :], lv[1].t[:, :], pvc(q0 + 1), sc[1].t[:, :], ALU.mult, ALU.add, [lv[1].b, sc[1].b], [sc[1].b])
                    STT(sc[1].t[:, :], lv[2].t[:, :], pvc(q0 + 2), sc[1].t[:, :], ALU.mult, ALU.add, [lv[2].b, sc[1].b], [sc[1].b])
                    TS("dve", sc[1].t[:, :], sc[1].t[:, :], pvc(q0 + 3), None, ALU.add, None, [sc[1].b, pv.b], [sc[1].b])
                    TS("dve", qtmp.t[:, :], sc[1].t[:, 0:128], pvc(selA), None, ALU.mult, None, [sc[1].b, pv.b], [qtmp.b])
                    STT(QA[v].t[:, sig * 128:(sig + 1) * 128], sc[1].t[:, 128:256], pvc(selB), qtmp.t[:, :],
                        ALU.mult, ALU.add, [sc[1].b, qtmp.b, pv.b], [QA[v].bs[sig]])
        S.barrier()
        if stop == "Fb":
            dump([(KF.t[:, 0, 0:128], KF.bs, 128, False), (KF.t[:, 3, 3968:4096], KF.bs, 128, False),
                  (KA.t[:, 1000:1128], KA.bs, 128, False), (QF.t[:, 1, 640:768], QF.bs, 128, False),
                  (QA[1].t[:, 640:768], QA[1].bs, 128, False), (VF.t[:, 17, 0:130], VF.bs, 130, False)])
            return
        with ExitStack() as ea:
            Pr = sb(ea, "Pr", [128, 4, 512], BF16, 4)
            Y = sb(ea, "Yf", [128, 512], BF16)
            rec = sb(ea, "recf", [128, 1], F32)
            stF = {"pi": 0}
            for sigma in range(NSLOT):
                e_par = sigma % 2
                ntile = 2 * sigma + 2
                qs = slice(sigma * 128, (sigma + 1) * 128)

                def front(h, jb, sigma=sigma, e_par=e_par, ntile=ntile, qs=qs):
                    pr, p0 = h // 2, (h % 2) * 64
                    vq, base = h % 3, 32 * (h // 3)
                    nj = min(4, ntile - jb)
                    bk = gbank()
                    for jj in range(nj):
                        j = jb + jj
                        ks = slice(j * 128, (j + 1) * 128)
                        cols = bk.t[:, jj * 128:(jj + 1) * 128]
                        hm = j >= 2 * sigma
                        MM(cols, KF.t[p0:p0 + 64, pr, ks], QF.t[p0:p0 + 64, pr, qs], True, False,
                           [KF.bs[j // 2], QF.bs[sigma]], [bk.b])
                        MM(cols, KA.t[base:base + 18, ks], QA[vq].t[base:base + 18, qs], False, not hm,
                           [KA.bs[j // 2], QA[vq].bs[sigma]], [bk.b])
                        if hm:
                            mi = e_par * 2 + (j - 2 * sigma)
                            MM(cols, identb.t[:, :], maskFb.t[:, mi * 128:(mi + 1) * 128], False, True,
                               [identb.b, maskFb.b], [bk.b])
                    sl = stF["pi"] % 4
                    stF["pi"] += 1
                    ACTV(Pr.t[:, sl, :nj * 128], bk.t[:, :nj * 128], AF.Exp, [bk.b], [Pr.bs[sl]], scale=0.125)
                    return sl

                def back(h, jb, sl, ntile=ntile):
                    Ob = psb[OB0 + (h % 2)]
                    nj = min(4, ntile - jb)
                    for jj in range(nj):
                        j = jb + jj
                        MM(Ob.t[:, 0:65], Pr.t[:, sl, jj * 128:(jj + 1) * 128], VF.t[:, j, h * 65:(h + 1) * 65],
                           j == 0, j == ntile - 1, [Pr.bs[sl], VF.bs[j // 2]], [Ob.b])

                run_attention(ntile, front, back,
                              lambda h: pv_norm(psb[OB0 + (h % 2)], Y, h, rec),
                              lambda sigma=sigma: attn_finish(Y, yfoxT, sigma), 0)

    S.barrier()
    if stop == "F":
        dump([(yfoxT.t[:, fc, sg_ * 128:(sg_ + 1) * 128], [yfoxT.b], 128, False) for sg_ in (0, 5) for fc in range(4)])
        return
    with ExitStack() as es:
        KD = sb(es, "KD", [128, 4, S_LEN], BF16, 16)
        VD = sb(es, "VD", [128, NT, 8 * 65], BF16, 16)
        KI = sb(es, "KI", [128, S_LEN], BF16, 16)
        QD = sb(es, "QD", [128, 4, NSLOT * 128], BF16, 16)
        QI = sb(es, "QI", [128, 2, NSLOT * 128], BF16, 16)
        WQ = sb(es, "WQ", [128, NSLOT * 4], F32, 16)
        MSET("pool", VD.t[:, :, :].rearrange("p t (h e) -> p t h e", h=8)[:, :, :, 64:65], 1.0, VD.bs)
        with ExitStack() as eb:
            NW = 1536 + 256 + 128 + 4
            wD = sb(eb, "wD", [128, 8, NW], BF16)
            load_w(wD.t[:, :, 0:1536], w_in[:, QD0:QD0 + 1536], [wD.b])
            load_w(wD.t[:, :, 1536:1792], w_in[:, QI0:QI0 + 256], [wD.b])
            load_w(wD.t[:, :, 1792:1920], wkidup[:, :], [wD.b])
            load_w(wD.t[:, :, 1920:1924], w_in[:, WI0:WI0 + 4], [wD.b])
            tl = {"xt": sb(eb, "xt", [128, 2, D], F32, 2), "xT": sb(eb, "xT", [128, 8, 256], F32, 8),
                  "sq": sb(eb, "sq", [128, 2, 256], F32, 2), "tmp": sb(eb, "tmp", [128, 2, 256], F32, 2)}
            tl2 = dict(tl)
            tl2["xT"] = sb(eb, "xTb", [128, 8, 256], F32, 8)
            tls = [tl, tl2]
            rstd = sb(eb, "rstd", [128, 256], F32)
            hTs = [sb(eb, "hT%d" % i, [128, 8, 256], BF16, 8) for i in range(2)]
            qtmp = sb(eb, "qtmp", [128, 128], F32)
            T = 256
            for blk in range(16):
                hT = hTs[blk % 2]
                tl = tls[blk % 2]
                src = load_T(tl, [xall[blk * 256 + tt * 128: blk * 256 + (tt + 1) * 128, :] for tt in range(2)], T, False)
                stats_rstd(tl, src, T, rstd)
                modulate(tl, T, rstd, A1, SH1, hT)
                sig = blk
                selA, selB = par_sel(blk)
                for pr in range(4):
                    bk = projT(lambda c, pr=pr: wD.t[:, c, 512 + pr * 128: 512 + (pr + 1) * 128], wD.b, hT, T, 128)
                    CP("act", KD.t[:, pr, blk * 256:(blk + 1) * 256], bk.t[:, :T], [bk.b, wD.b], [KD.bs[blk]])
                bk = projT(lambda c: wD.t[:, c, 1792:1920], wD.b, hT, T, 128)
                CP("act", KI.t[:, blk * 256:(blk + 1) * 256], bk.t[:, :T], [bk.b, wD.b], [KI.bs[blk]])
                for pr in range(4):
                    bk = projT(lambda c, pr=pr: wD.t[:, c, pr * 128:(pr + 1) * 128], wD.b, hT, T, 128)
                    TS("dve", qtmp.t[:, :], bk.t[:, 0:128], pvc(selA), None, ALU.mult, None, [bk.b, pv.b, wD.b], [qtmp.b])
                    STT(QD.t[:, pr, sig * 128:(sig + 1) * 128], bk.t[:, 128:256], pvc(selB), qtmp.t[:, :],
                        ALU.mult, ALU.add, [bk.b, qtmp.b, pv.b], [QD.bs[sig]])
                for ch in range(2):
                    bk = projT(lambda c, ch=ch: wD.t[:, c, 1536 + ch * 128: 1536 + (ch + 1) * 128], wD.b, hT, T, 128)
                    TS("dve", qtmp.t[:, :], bk.t[:, 0:128], pvc(selA), None, ALU.mult, None, [bk.b, pv.b, wD.b], [qtmp.b])
                    STT(QI.t[:, ch, sig * 128:(sig + 1) * 128], bk.t[:, 128:256], pvc(selB), qtmp.t[:, :],
                        ALU.mult, ALU.add, [bk.b, qtmp.b, pv.b], [QI.bs[sig]])
                for tt in range(2):
                    bk = gbank()
                    for c in range(8):
                        MM(bk.t[:, :512], hT.t[:, c, tt * 128:(tt + 1) * 128], wD.t[:, c, 1024:1536], c == 0, c == 7,
                           [hT.bs[c], wD.b], [bk.b])
                    tile_i = blk * 2 + tt
                    CP("act", VD.t[:, tile_i, :].rearrange("p (h e) -> p h e", h=8)[:, :, 0:64],
                       bk.t[:, :512].rearrange("p (h d) -> p h d", h=8), [bk.b], [VD.bs[blk]])
                bk = gbank()
                for tt in range(2):
                    for c in range(8):
                        MM(bk.t[:, tt * 4:(tt + 1) * 4], hT.t[:, c, tt * 128:(tt + 1) * 128], wD.t[:, c, 1920:1924],
                           c == 0, c == 7, [hT.bs[c], wD.b], [bk.b])
                sA = PV_PAR0S if selA == PV_PAR0 else PV_PAR1S
                sB = PV_PAR0S if selB == PV_PAR0 else PV_PAR1S
                TS("dve", qtmp.t[:, 0:4], bk.t[:, 0:4], pvc(sA), None, ALU.mult, None, [bk.b, pv.b], [qtmp.b])
                STT(WQ.t[:, sig * 4:(sig + 1) * 4], bk.t[:, 4:8], pvc(sB), qtmp.t[:, 0:4], ALU.mult, ALU.add,
                    [bk.b, qtmp.b, pv.b], [WQ.bs[sig]])
        S.barrier()
        if stop == "Db":
            dump([(KD.t[:, 2, 1024:1152], KD.bs, 128, False), (KI.t[:, 1024:1152], KI.bs, 128, False),
                  (QD.t[:, 1, 640:768], QD.bs, 128, False), (QI.t[:, 1, 640:768], QI.bs, 128, False),
                  (WQ.t[:, 0:64], WQ.bs, 64, True), (VD.t[:, 17, 0:130], VD.bs, 130, False)])
            return
        with ExitStack() as ea:
            isc = sb(ea, "isc", [128, S_LEN], F32)
            mq = sb(ea, "mq", [128, S_LEN], BF16)
            maskTs = [sb(ea, "maskT%d" % i, [128, NT, 128], BF16) for i in range(2)]
            B8 = sb(ea, "B8", [128, 8 * 6 * 128], BF16)
            maskIs = sb(ea, "maskIs", [128, 512], F32)
            rl = sb(ea, "rl", [128, 2, 512], F32, 2)
            Pr = sb(ea, "Prd", [128, 4, 512], BF16, 4)
            Pm = sb(ea, "Pmd", [128, 4, 512], BF16, 4)
            Y = sb(ea, "Yd", [128, 512], BF16)
            recs = [sb(ea, "recd%d" % i, [128, 1], F32) for i in range(2)]
            small = sb(ea, "small", [128, 8], F32)
            steps = sb(ea, "steps", [128, NBIS + 1], F32)
            p2 = sb(ea, "p2", [128, NBIS + 1], F32)
            S.dma(maskIs.t[:], maskId[:, :], writes=[maskIs.b])
            for i in range(NBIS + 1):
                MSET("dve", p2.t[:, i:i + 1], 2.0 ** (-i), [p2.b])
            with ExitStack() as et:
                bst = sb(et, "bst", [128, 768], F32)
                for h in range(8):
                    S.dma(bst.t[:], bmatd[:, h * 768:(h + 1) * 768], writes=[bst.b])
                    TS("dve", B8.t[:, h * 768:(h + 1) * 768], bst.t[:, :], pvc(PV_B31 + h), 8.0, ALU.subtract, ALU.mult,
                       [bst.b, pv.b], [B8.b])
            S.barrier()
            if stop == "D0":
                dump([(B8.t[:, 0:512], [B8.b], 512, False), (p2.t[:, :], [p2.b], NBIS, True)])
                return
            st = {"pi": 0, "ri": 0}
            small = [small, sb(ea, "small2", [128, 8], F32)]
            steps = [steps, sb(ea, "steps2", [128, NBIS + 1], F32)]

            def prep_steps(sigma):
                out = []
                e_par = sigma % 2
                ntile = 2 * sigma + 2
                nk = ntile * 128
                qs = slice(sigma * 128, (sigma + 1) * 128)
                mT = maskTs[sigma % 2]
                sm, stp = small[sigma % 2], steps[sigma % 2]
                R, lo, thr, cnt, dd = (sm.t[:, i:i + 1] for i in range(5))

                def idx(k0, wd, hi):
                    kbufs = [KI.bs[(k0 + t * 256) // 256] for t in range((wd + 255) // 256)]
                    p0, ch = (hi % 2) * 64, hi // 2
                    bk = gbank()
                    MM(bk.t[:, :wd], QI.t[p0:p0 + 64, ch, qs], KI.t[p0:p0 + 64, k0:k0 + wd], True, True,
                       [QI.bs[sigma]] + kbufs, [bk.b])
                    wcol = WQ.t[:, sigma * 4 + hi: sigma * 4 + hi + 1]
                    if hi == 0:
                        TS("dve", isc.t[:, k0:k0 + wd], bk.t[:, :wd], 0.0, wcol, ALU.max, ALU.mult,
                           [bk.b, WQ.bs[sigma]], [isc.b])
                    else:
                        sl = st["ri"] % 2
                        st["ri"] += 1
                        ACTV(rl.t[:, sl, :wd], bk.t[:, :wd], AF.Relu, [bk.b], [rl.bs[sl]])
                        STT(isc.t[:, k0:k0 + wd], rl.t[:, sl, :wd], wcol, isc.t[:, k0:k0 + wd], ALU.mult, ALU.add,
                            [rl.bs[sl], WQ.bs[sigma], isc.b], [isc.b])
                for k0 in range(0, nk, 512):
                    wd = min(512, nk - k0)
                    for hi in range(4):
                        out.append(lambda k0=k0, wd=wd, hi=hi: idx(k0, wd, hi))

                def init():
                    S.op("dve", lambda e: e.tensor_reduce(out=R, in_=isc.t[:, :nk], axis=AX.X, op=ALU.max,
                                                          apply_absolute_value=True), [isc.b], [sm.b])
                    TT("dve", isc.t[:, 2 * sigma * 128: nk], isc.t[:, 2 * sigma * 128: nk],
                       maskIs.t[:, e_par * 256:(e_par + 1) * 256], ALU.add, [isc.b, maskIs.b], [isc.b])
                    TS("dve", stp.t[:, :], p2.t[:, :], R, None, ALU.mult, None, [p2.b, sm.b], [stp.b])
                    TS("dve", thr, R, 0.0, None, ALU.mult, None, [sm.b], [sm.b])
                out.append(init)

                def bis(it):
                    TS("dve", mq.t[:, :nk], isc.t[:, :nk], thr, 0.0, ALU.is_ge, ALU.add, [isc.b, sm.b], [mq.b, sm.b],
                       accum_out=cnt)
                    STT(dd, cnt, 256.0, stp.t[:, it:it + 1], ALU.is_ge, ALU.mult, [sm.b, stp.b], [sm.b])
                    TS("dve", thr, thr, dd, stp.t[:, it + 1:it + 2], ALU.add, ALU.subtract, [sm.b, stp.b], [sm.b])
                for it in range(NBIS):
                    out.append(lambda it=it: bis(it))
                out.append(lambda: TS("dve", lo, thr, stp.t[:, NBIS:NBIS + 1], None, ALU.subtract, None, [sm.b, stp.b], [sm.b]))
                out.append(lambda: TS("dve", mq.t[:, :nk], isc.t[:, :nk], lo, None, ALU.is_ge, None, [isc.b, sm.b], [mq.b]))

                def tr(jb):
                    nj = min(4, ntile - jb)
                    psT = psTs[(jb // 4) % 2]
                    for jj in range(nj):
                        j = jb + jj
                        TR(psT.t[:, jj * 128:(jj + 1) * 128], mq.t[:, j * 128:(j + 1) * 128],
                           identb.t[:, :], [mq.b, identb.b], [psT.b])
                    CP("act", mT.t[:, jb:jb + nj, :],
                       psT.t[:, 0:nj * 128].rearrange("p (j q) -> p j q", j=nj), [psT.b], [mT.b])
                post = [(lambda jb=jb: tr(jb)) for jb in range(0, ntile, 4)]
                return out, post

            def attn_slot(sigma):
                e_par = sigma % 2
                ntile = 2 * sigma + 2
                qs = slice(sigma * 128, (sigma + 1) * 128)
                mT = maskTs[sigma % 2]

                def front(h, jb):
                    pr, p0 = h // 2, (h % 2) * 64
                    nj = min(4, ntile - jb)
                    bk = gbank()
                    for jj in range(nj):
                        j = jb + jj
                        ks = slice(j * 128, (j + 1) * 128)
                        cols = bk.t[:, jj * 128:(jj + 1) * 128]
                        wh = j - (2 * sigma - 1)
                        near = 0 <= wh <= 2
                        MM(cols, KD.t[p0:p0 + 64, pr, ks], QD.t[p0:p0 + 64, pr, qs], True, not near,
                           [KD.bs[j // 2], QD.bs[sigma]], [bk.b])
                        if near:
                            bi = (h * 6 + e_par * 3 + wh) * 128
                            MM(cols, identb.t[:, :], B8.t[:, bi:bi + 128], False, True, [identb.b, B8.b], [bk.b])
                    sl = st["pi"] % 4
                    st["pi"] += 1
                    ACTV(Pr.t[:, sl, :nj * 128], bk.t[:, :nj * 128], AF.Exp, [bk.b, pv.b], [Pr.bs[sl]],
                         scale=0.125, bias=pvc(PV_B31 + h))
                    TT("pool", Pm.t[:, sl, :nj * 128], Pr.t[:, sl, :nj * 128],
                       mT.t[:, jb:jb + nj, :].rearrange("p j q -> p (j q)"), ALU.mult,
                       [Pr.bs[sl], mT.b], [Pm.bs[sl]])
                    return sl

                def back(h, jb, sl):
                    Ob = psb[OB0 + (h % 2)]
                    nj = min(4, ntile - jb)
                    for jj in range(nj):
                        j = jb + jj
                        MM(Ob.t[:, 0:65], Pm.t[:, sl, jj * 128:(jj + 1) * 128], VD.t[:, j, h * 65:(h + 1) * 65],
                           j == 0, j == ntile - 1, [Pm.bs[sl], VD.bs[j // 2]], [Ob.b])

                run_attention(ntile, front, back,
                              lambda h: pv_norm_act(psb[OB0 + (h % 2)], Y, h, recs[h % 2]),
                              lambda: attn_finish(Y, ydsaT, sigma), 1, look=2)

            pre, post = prep_steps(0)
            for fn in pre + post:
                fn()
            for sigma in range(_DBG_DSLOTS):
                pre, post = prep_steps(sigma + 1) if sigma + 1 < _DBG_DSLOTS else ([], [])
                for fn in pre:
                    fn()
                attn_slot(sigma)
                for fn in post:
                    fn()
    S.barrier()
    if stop == "D":
        dump([(ydsaT.t[:, fc, sg_ * 128:(sg_ + 1) * 128], [ydsaT.b], 128, False) for sg_ in (0, 5) for fc in range(4)])
        return
    with ExitStack() as es:
        wbf = sb(es, "wbf", [128, 4, D], BF16)
        wbd = sb(es, "wbd", [128, 4, D], BF16)
        wo = sb(es, "wo", [128, 8, D], BF16)
        S.dma(wbf.t[:], wbfb.rearrange("(c p) n -> p c n", p=128), reads=[convb], writes=[wbf.b])
        S.dma(wbd.t[:], wbdb.rearrange("(c p) n -> p c n", p=128), reads=[convb], writes=[wbd.b])
        S.dma(wo.t[:, :, :], wob.rearrange("(c p) n -> p c n", p=128), reads=[convb], writes=[wo.b])
        tl = {"xt": sb(es, "xt", [128, 4, D], F32, 4), "xT": sb(es, "xT", [128, 8, 512], F32, 8),
              "sq": sb(es, "sq", [128, 2, 512], F32, 2), "tmp": sb(es, "tmp", [128, 2, 512], F32, 2)}
        xT = tl["xT"]
        rstd = sb(es, "rstd", [128, 512], F32)
        hT = sb(es, "hT", [128, 8, 512], BF16, 8)
        mg = sb(es, "mg", [128, 8, 512], BF16, 8)
        aT = sb(es, "aT", [128, 32, 512], BF16, 32)
        wg = sb(es, "wg", [128, 2, 8, 256], BF16, 2)
        sg = sb(es, "sg", [128, 2, 512], F32, 2)
        tg = sb(es, "tg", [128, 2, 512], F32, 2)
        w1t = sb(es, "w1t", [128, 2, 8, 512], BF16, 2)
        w2t = sb(es, "w2t", [128, 4, 512], BF16, 4)
        rr = sb(es, "rr", [128, 2, 512], F32, 2)
        ot = sb(es, "ot", [128, 2, D], F32, 2)
        T = 512
        w2i = 0
        last_out = []
        for g in range(4):
            gs = slice(g * 512, (g + 1) * 512)
            src = load_T(tl, [xown[g * 512 + tt * 128: g * 512 + (tt + 1) * 128, :] for tt in range(4)], T, True)
            stats_rstd(tl, src, T, rstd)
            modulate(tl, T, rstd, A1, SH1, hT)
            for oc in range(8):
                sl = oc % 2
                S.dma(wg.t[:, sl, :, 0:128], wgb[:, oc * 128:(oc + 1) * 128].rearrange("(c p) n -> p c n", p=128),
                      reads=[convb], writes=[wg.bs[sl]])
                S.dma(wg.t[:, sl, :, 128:256], wgb[:, 1024 + oc * 128: 1024 + (oc + 1) * 128].rearrange("(c p) n -> p c n", p=128),
                      reads=[convb], writes=[wg.bs[sl]])
                for br, (wb_, yT_) in enumerate(((wbf, yfoxT), (wbd, ydsaT))):
                    bk = gbank()
                    for c in range(8):
                        MM(bk.t[:, :T], wg.t[:, sl, c, br * 128:(br + 1) * 128], hT.t[:, c, :T], c == 0, c == 7,
                           [wg.bs[sl], hT.bs[c]], [bk.b])
                    ACTV(sg.t[:, br, :], bk.t[:, :T], AF.Sigmoid, [bk.b], [sg.bs[br]])
                    bk2 = gbank()
                    for fc in range(4):
                        MM(bk2.t[:, :T], wb_.t[:, fc, oc * 128:(oc + 1) * 128], yT_.t[:, fc, gs], fc == 0, fc == 3,
                           [wb_.b, yT_.b], [bk2.b])
                    TT("dve", tg.t[:, br, :], sg.t[:, br, :], bk2.t[:, :T], ALU.mult, [sg.bs[br], bk2.b], [tg.bs[br]])
                TT("pool", mg.t[:, oc, :], tg.t[:, 0, :], tg.t[:, 1, :], ALU.add, [tg.bs[0], tg.bs[1]], [mg.bs[oc]])
            for oc in range(8):
                bk = gbank()
                for c in range(8):
                    MM(bk.t[:, :T], wo.t[:, c, oc * 128:(oc + 1) * 128], mg.t[:, c, :], c == 0, c == 7,
                       [wo.b, mg.bs[c]], [bk.b])
                STT(xT.t[:, oc, :], bk.t[:, :T], G1(oc), xT.t[:, oc, :], ALU.mult, ALU.add, [bk.b, xT.bs[oc]] + cb, [xT.bs[oc]])
            stats_rstd(tl, [(xT.t[:, c, :], [xT.bs[c]]) for c in range(8)], T, rstd)
            modulate(tl, T, rstd, A2, SH2, hT)
            for fb in range(8):
                sl = fb % 2
                S.dma(w1t.t[:, sl, :, :], w1b[:, fb * 512:(fb + 1) * 512].rearrange("(c p) n -> p c n", p=128),
                      reads=[convb], writes=[w1t.bs[sl]])
                for f4 in range(4):
                    fcn = fb * 4 + f4
                    bk = gbank()
                    for c in range(8):
                        MM(bk.t[:, :T], w1t.t[:, sl, c, f4 * 128:(f4 + 1) * 128], hT.t[:, c, :], c == 0, c == 7,
                           [w1t.bs[sl], hT.bs[c]], [bk.b])
                    rs = fcn % 2
                    ACTV(rr.t[:, rs, :], bk.t[:, :T], AF.Relu, [bk.b], [rr.bs[rs]])
                    TT("pool", aT.t[:, fcn, :], rr.t[:, rs, :], rr.t[:, rs, :], ALU.mult, [rr.bs[rs]], [aT.bs[fcn]])
            for half in range(2):
                accs = [psb[2], psb[3], psb[4], psb[5]]
                for fcn in range(32):
                    sl = w2i % 4
                    w2i += 1
                    S.dma(w2t.t[:, sl, :], w2b[fcn * 128:(fcn + 1) * 128, half * 512:(half + 1) * 512],
                          reads=[convb], writes=[w2t.bs[sl]])
                    for o4 in range(4):
                        MM(accs[o4].t[:, :T], w2t.t[:, sl, o4 * 128:(o4 + 1) * 128], aT.t[:, fcn, :], fcn == 0, fcn == 31,
                           [w2t.bs[sl], aT.bs[fcn]], [accs[o4].b])
                for o4 in range(4):
                    oc = half * 4 + o4
                    STT(xT.t[:, oc, :], accs[o4].t[:, :T], G2(oc), xT.t[:, oc, :], ALU.mult, ALU.add,
                        [accs[o4].b, xT.bs[oc]] + cb, [xT.bs[oc]])
            stats_rstd(tl, [(xT.t[:, c, :], [xT.bs[c]]) for c in range(8)], T, rstd)
            for c in range(8):
                STT(xT.t[:, c, :], xT.t[:, c, :], pv.t[:, PV_GF + c:PV_GF + c + 1], rstd.t[:, :T], ALU.mult, ALU.mult,
                    [xT.bs[c], rstd.b, pv.b], [xT.bs[c]])
            for tt in range(4):
                osl = tt % 2
                for cbk in range(2):
                    bk = gbank()
                    for c4 in range(4):
                        c = cbk * 4 + c4
                        TR(bk.t[:, c4 * 128:(c4 + 1) * 128], xT.t[:, c, tt * 128:(tt + 1) * 128], identf.t[:, :],
                           [xT.bs[c], identf.b], [bk.b])
                    CP("act" if cbk == 0 else "dve", ot.t[:, osl, cbk * 512:(cbk + 1) * 512], bk.t[:, :512], [bk.b], [ot.bs[osl]])
                t = S.dma(outd[g * 512 + tt * 128: g * 512 + (tt + 1) * 128, :], ot.t[:, osl, :], reads=[ot.bs[osl]])
                last_out.append(t)
        S.wait("sp", last_out)
    ES.close()
    return nc


def _t5_bucket(dist):
    n = np.maximum(dist, 0)
    max_exact = 16
    nf = np.maximum(n, 1).astype(np.float32)
    large = max_exact + (np.log(nf / np.float32(max_exact)) / np.float32(math.log(128 / max_exact))
                         * np.float32(32 - max_exact)).astype(np.int32)
    large = np.minimum(large, 31)
    return np.where(n < max_exact, n, large)


def _own_tiles(p):
    tiles = []
    for m in range(8):
        tiles.append(4 * m + p)
        tiles.append(4 * m + 3 - p)
    return tiles


_NC_CACHE = {}


def kernel(x, c, w_ada, b_ada, g_norm1, w_in, b_forget, rel_bias, w_branch_fox, w_branch_dsa, w_out,
           g_norm2, w_mlp1, w_mlp2, g_final):
    f32 = np.float32
    x = np.asarray(x, f32)
    c = np.asarray(c, f32)
    w_in = np.ascontiguousarray(np.asarray(w_in, f32))
    b_ada = np.asarray(b_ada, f32)
    rel_bias = np.asarray(rel_bias, f32)
    b_forget = np.asarray(b_forget, f32)
    col = lambda v: np.ascontiguousarray(np.asarray(v, f32).reshape(-1, 128).T)

    wfpad = np.zeros((D, 128), f32)
    bfcol = np.zeros((128, 1), f32)
    for h in range(8):
        base = 32 * (h // 3) + 6 * (h % 3)
        for r in range(6):
            wfpad[:, base + r] = w_in[:, FL0 + h]
            bfcol[base + r, 0] = b_forget[h]
    wkidup = np.ascontiguousarray(np.concatenate([w_in[:, KI0:KI0 + 64], w_in[:, KI0:KI0 + 64]], axis=1))
    ident = np.eye(128, dtype=f32)
    rows = np.arange(128)
    rmod = rows % 32
    rv, rr = rmod // 6, rmod % 6
    act = rmod < 18
    selK = np.stack([act & (rr == 3), act & (rr == 4), act & (rr == 5), act & (rr < 3)], 1).astype(f32)
    selQ = np.zeros((128, 12), f32)
    for v in range(3):
        mine = act & (rv == v)
        selQ[:, 4 * v + 0] = -(mine & (rr == 0)).astype(f32)
        selQ[:, 4 * v + 1] = -(mine & (rr == 1)).astype(f32)
        selQ[:, 4 * v + 2] = -(mine & (rr == 2)).astype(f32)
        selQ[:, 4 * v + 3] = (mine & (rr >= 3)).astype(f32)

    in_maps = []
    own = []
    for core in range(8):
        b, p = core // 2, core % 2
        tiles = _own_tiles(p)
        own.append(tiles)
        xb = x[b]
        xo = np.ascontiguousarray(xb.reshape(NT, 128, D)[tiles].reshape(NSLOT * 128, D))
        pvv = np.zeros((128, NPV), f32)
        pvv[:, PV_C:PV_C + 8] = col(c[b])
        pvv[:, PV_BADA:PV_BADA + 48] = col(b_ada)
        pvv[:, PV_G1:PV_G1 + 8] = col(g_norm1)
        pvv[:, PV_G2:PV_G2 + 8] = col(g_norm2)
        pvv[:, PV_GF:PV_GF + 8] = col(g_final)
        pvv[:, PV_PAR0] = 1.0 if p == 0 else 0.0
        pvv[:, PV_PAR1] = 1.0 if p == 1 else 0.0
        pvv[:, PV_PAR0S] = 0.0625 if p == 0 else 0.0
        pvv[:, PV_PAR1S] = 0.0625 if p == 1 else 0.0
        pvv[:, PV_BF:PV_BF + 1] = bfcol
        pvv[:, PV_SK:PV_SK + 4] = selK
        pvv[:, PV_SQ:PV_SQ + 12] = selQ
        pvv[:, PV_B31:PV_B31 + 8] = rel_bias[31][None, :]
        pvv[:, PV_EPS] = 1e-6
        kk = rows[:, None]
        qq = rows[None, :]
        maskF = np.zeros((128, 4, 128), f32)
        maskI = np.zeros((128, 2, 256), f32)
        bmat = np.zeros((128, 8, 2, 3, 128), f32)
        for e in range(2):
            qt = p if e == 0 else 3 - p
            for which in range(2):
                kt = 2 * e + which
                ok = (kt * 128 + kk) <= (qt * 128 + qq)
                maskF[:, e * 2 + which, :] = np.where(ok, 0.0, NEG)
                maskI[:, e, which * 128:(which + 1) * 128] = np.where(ok.T, 0.0, -1e30)
            for w3 in range(3):
                kt = 2 * e - 1 + w3
                dist = (qt * 128 + qq) - (kt * 128 + kk)
                bk = _t5_bucket(dist)
                bmat[:, :, e, w3, :] = np.transpose(rel_bias[bk], (0, 2, 1))
        in_maps.append({
            "xall": np.ascontiguousarray(xb), "xown": xo, "pv": pvv,
            "w_ada": np.asarray(w_ada, f32), "w_in": w_in, "wfpad": wfpad, "wkidup": wkidup, "ident": ident,
            "maskF": np.ascontiguousarray(maskF.reshape(128, 512)),
            "maskI": np.ascontiguousarray(maskI.reshape(128, 512)),
            "bmat": np.ascontiguousarray(bmat.reshape(128, 8 * 6 * 128)),
            "w_branch_fox": np.asarray(w_branch_fox, f32), "w_branch_dsa": np.asarray(w_branch_dsa, f32),
            "w_out": np.asarray(w_out, f32), "w_mlp1": np.asarray(w_mlp1, f32), "w_mlp2": np.asarray(w_mlp2, f32),
        })
    if "nc" not in _NC_CACHE:
        _NC_CACHE["nc"] = build_program()
    nc = _NC_CACHE["nc"]
    res = run_bass_kernel_spmd(nc, in_maps, core_ids=list(range(8)))
    out = np.zeros((4, S_LEN, D), f32)
    for core in range(8):
        b = core // 2
        o = np.asarray(res.results[core]["out"], f32).reshape(NSLOT, 128, D)
        out[b].reshape(NT, 128, D)[own[core]] = o
    return out
```

```python
import math
from contextlib import ExitStack
import numpy as np
import concourse.bass as bass
import concourse.mybir as mybir
from concourse.bass_utils import run_bass_kernel_spmd

F32 = mybir.dt.float32
BF16 = mybir.dt.bfloat16
ALU = mybir.AluOpType
AF = mybir.ActivationFunctionType
AX = mybir.AxisListType

D = 1024
S_LEN = 4096
NT = 32
NSLOT = 16
HD = 64
NBIS = 12
NEG = -30000.0
_DBG_DSLOTS = NSLOT
QF0, KF0, VF0, FL0, QD0, KD0, VD0, QI0, KI0, WI0, GF0, GD0 = (
    0, 512, 1024, 1536, 1544, 2056, 2568, 3080, 3336, 3400, 3404, 4428)
PV_C, PV_BADA, PV_G1, PV_G2, PV_GF = 0, 8, 56, 64, 72
PV_PAR0, PV_PAR1, PV_PAR0S, PV_PAR1S = 80, 81, 82, 83
PV_BF = 84
PV_SK = 86
PV_SQ = 90
PV_B31 = 102
PV_EPS = 110
NPV = 112


class Buf:
    __slots__ = ("w", "r")

    def __init__(self):
        self.w = None
        self.r = []


class Sched:
    ENGS = ("pe", "act", "dve", "pool", "sp")

    def __init__(self, nc, n_dma_sems=32):
        self.nc = nc
        self.h = {"pe": nc.tensor, "act": nc.scalar, "dve": nc.vector,
                  "pool": nc.gpsimd, "sp": nc.sync}
        self.sem = {e: nc.alloc_semaphore("c_" + e) for e in ("pe", "act", "dve", "pool")}
        self.cnt = {e: 0 for e in self.sem}
        self.known = {e: {} for e in self.ENGS}
        self.dsem = {q: [nc.alloc_semaphore("d%s%d" % (q, i)) for i in range(n_dma_sems)] for q in ("sp", "pool")}
        self.dcnt = {q: [0] * n_dma_sems for q in ("sp", "pool")}
        self.dnext = {"sp": 0, "pool": 0}

    def _semh(self, key):
        return self.sem[key] if isinstance(key, str) else self.dsem[key[1]][key[2]]

    def wait(self, eng, tickets):
        best = {}
        for t in tickets:
            if t is None:
                continue
            k, v = t
            if k == eng and eng == "pe":
                continue
            if best.get(k, 0) < v:
                best[k] = v
        kn = self.known[eng]
        for k, v in best.items():
            if kn.get(k, 0) >= v:
                continue
            self.h[eng].wait_ge(self._semh(k), v)
            kn[k] = v

    def _deps(self, reads, writes):
        deps = []
        for b in reads:
            deps.append(b.w)
        for b in writes:
            deps.append(b.w)
            deps.extend(b.r)
        return deps

    def _commit(self, t, reads, writes):
        for b in reads:
            b.r.append(t)
        for b in writes:
            b.w = t
            b.r = []

    def op(self, eng, fn, reads=(), writes=()):
        self.wait(eng, self._deps(reads, writes))
        ins = fn(self.h[eng])
        self.cnt[eng] += 1
        ins.then_inc(self.sem[eng], 1)
        t = (eng, self.cnt[eng])
        self._commit(t, reads, writes)
        return t

    def dma(self, out, in_, reads=(), writes=(), q="sp"):
        deps = self._deps(reads, writes)
        i = self.dnext[q]
        self.dnext[q] = (i + 1) % len(self.dsem[q])
        if self.dcnt[q][i] > 0:
            deps.append((("d", q, i), self.dcnt[q][i]))
        self.wait(q, deps)
        self.dcnt[q][i] += 16
        self.h[q].dma_start(out=out, in_=in_).then_inc(self.dsem[q][i], 16)
        t = (("d", q, i), self.dcnt[q][i])
        self._commit(t, reads, writes)
        return t


def _barrier(self):
    tickets = [(e, c) for e, c in self.cnt.items() if c > 0]
    tickets += [(("d", q, i), v) for q in self.dcnt for i, v in enumerate(self.dcnt[q]) if v > 0]
    for e in self.ENGS:
        self.wait(e, tickets)


Sched.barrier = _barrier


class TB:
    def __init__(self, t, n=1):
        self.t = t
        self.b = Buf()
        self.bs = [Buf() for _ in range(n)]


class _Stop(Exception):
    pass


def build_program(stop=None):
    holder = {}
    try:
        _build(holder, stop)
    except _Stop:
        pass
    holder["ES"].close()
    return holder["nc"]


def _build(holder, stop):
    stop_sigma = 1
    if stop and "@" in stop:
        stop, ss = stop.split("@")
        stop_sigma = int(ss)
    nc = bass.Bass("TRN2", target_bir_lowering=False)
    holder["nc"] = nc
    dt = nc.dram_tensor
    xall = dt("xall", [S_LEN, D], F32, kind="ExternalInput").ap()
    xown = dt("xown", [NSLOT * 128, D], F32, kind="ExternalInput").ap()
    pvd = dt("pv", [128, NPV], F32, kind="ExternalInput").ap()
    w_ada = dt("w_ada", [D, 6 * D], F32, kind="ExternalInput").ap()
    w_in = dt("w_in", [D, 5452], F32, kind="ExternalInput").ap()
    wfpad = dt("wfpad", [D, 128], F32, kind="ExternalInput").ap()
    wkidup = dt("wkidup", [D, 128], F32, kind="ExternalInput").ap()
    identd = dt("ident", [128, 128], F32, kind="ExternalInput").ap()
    maskFd = dt("maskF", [128, 4 * 128], F32, kind="ExternalInput").ap()
    maskId = dt("maskI", [128, 2 * 256], F32, kind="ExternalInput").ap()
    bmatd = dt("bmat", [128, 8 * 6 * 128], F32, kind="ExternalInput").ap()
    wbfd = dt("w_branch_fox", [512, D], F32, kind="ExternalInput").ap()
    wbdd = dt("w_branch_dsa", [512, D], F32, kind="ExternalInput").ap()
    w_out = dt("w_out", [D, D], F32, kind="ExternalInput").ap()
    w_mlp1 = dt("w_mlp1", [D, 4 * D], F32, kind="ExternalInput").ap()
    w_mlp2 = dt("w_mlp2", [4 * D, D], F32, kind="ExternalInput").ap()
    outd = dt("out", [NSLOT * 128, D], F32, kind="ExternalOutput").ap()
    wgb = dt("wgb", [D, 2048], BF16, kind="Internal").ap()
    wbfb = dt("wbfb", [512, D], BF16, kind="Internal").ap()
    wbdb = dt("wbdb", [512, D], BF16, kind="Internal").ap()
    wob = dt("wob", [D, D], BF16, kind="Internal").ap()
    w1b = dt("w1b", [D, 4 * D], BF16, kind="Internal").ap()
    w2b = dt("w2b", [4 * D, D], BF16, kind="Internal").ap()

    S = Sched(nc)
    ES = ExitStack()
    convb = Buf()
    holder["ES"] = ES
    holder["S"] = S

    uid = {"i": 0}

    def sb(es, name, shape, dtype, n=1):
        uid["i"] += 1
        return TB(es.enter_context(nc.sbuf_tensor("s%d_%s" % (uid["i"], name), shape, dtype)), n)

    def MM(out, lhsT, rhs, start, stop, r, w):
        return S.op("pe", lambda e: e.matmul(out, lhsT=lhsT, rhs=rhs, start=start, stop=stop), r, w)

    def TR(out, in_, ident, r, w):
        return S.op("pe", lambda e: e.transpose(out=out, in_=in_, identity=ident), r, w)

    def ACTV(out, in_, func, r, w, **kw):
        return S.op("act", lambda e: e.activation(out=out, in_=in_, func=func, **kw), r, w)

    def TS(eng, out, in0, s1, s2, op0, op1, r, w, **kw):
        if op1 is None:
            return S.op(eng, lambda e: e.tensor_scalar(out=out, in0=in0, scalar1=s1, scalar2=None, op0=op0, **kw), r, w)
        return S.op(eng, lambda e: e.tensor_scalar(out=out, in0=in0, scalar1=s1, scalar2=s2, op0=op0, op1=op1, **kw), r, w)

    def STT(out, in0, scalar, in1, op0, op1, r, w):
        return S.op("dve", lambda e: e.scalar_tensor_tensor(out=out, in0=in0, scalar=scalar, in1=in1, op0=op0, op1=op1), r, w)

    def TT(eng, out, in0, in1, op, r, w):
        return S.op(eng, lambda e: e.tensor_tensor(out=out, in0=in0, in1=in1, op=op), r, w)

    def CP(eng, out, in_, r, w):
        if eng == "act":
            return ACTV(out, in_, AF.Copy, r, w)
        return S.op(eng, lambda e: e.tensor_copy(out=out, in_=in_), r, w)

    def MSET(eng, ap, val, w):
        return S.op(eng, lambda e: e.memset(ap, val), (), w)

    Stop = _Stop

    def dump(items):
        S.barrier()
        with ExitStack() as ed:
            stg = sb(ed, "dbgstg", [128, 1024], F32)
            c0 = 0
            for ap, bufs, ncols, isf in items:
                CP("dve", stg.t[:, c0:c0 + ncols], ap, bufs, [stg.b])
                c0 += ncols
            t = S.dma(outd[0:128, 0:c0], stg.t[:, 0:c0], reads=[stg.b])
            S.wait("sp", [t])

    psb = [TB(ES.enter_context(nc.psum_tensor("ps%d" % i, [128, 512], F32))) for i in range(6)]
    psTs = [TB(ES.enter_context(nc.psum_tensor("psT%d" % i, [128, 1024], BF16))) for i in range(2)]
    ring = {"i": 0}
    OB0, SSB = 3, 5

    def gbank():
        k = ring["i"]
        ring["i"] = (k + 1) % 3
        return psb[k]

    pv = sb(ES, "pv", [128, NPV], F32)
    identf = sb(ES, "identf", [128, 128], F32)
    identb = sb(ES, "identb", [128, 128], BF16)
    onesf = sb(ES, "onesf", [128, 512], F32)
    adaT = sb(ES, "adaT", [128, 48], F32)
    A12 = sb(ES, "A12", [128, 16], F32)
    yfoxT = sb(ES, "yfoxT", [128, 4, NSLOT * 128], BF16)
    ydsaT = sb(ES, "ydsaT", [128, 4, NSLOT * 128], BF16)

    S.dma(pv.t[:], pvd[:, :], writes=[pv.b])
    S.dma(identf.t[:], identd[:, :], writes=[identf.b])
    CP("dve", identb.t[:], identf.t[:], [identf.b], [identb.b])
    MSET("dve", onesf.t[:], 1.0, [onesf.b])

    def pvc(c, n=1):
        return pv.t[:, c:c + n]

    with ExitStack() as es:
        wa = sb(es, "wa", [128, 4, 8, 512], F32, 4)
        acc = psb[SSB]
        for cbk in range(12):
            sl = cbk % 4
            S.dma(wa.t[:, sl, :, :], w_ada[:, cbk * 512:(cbk + 1) * 512].rearrange("(c p) n -> p c n", p=128),
                  writes=[wa.bs[sl]])
            for j4 in range(4):
                j = cbk * 4 + j4
                for kc in range(8):
                    MM(acc.t[:, j:j + 1], wa.t[:, sl, kc, j4 * 128:(j4 + 1) * 128], pv.t[:, PV_C + kc:PV_C + kc + 1],
                       kc == 0, kc == 7, [wa.bs[sl], pv.b], [acc.b])
        TT("dve", adaT.t[:, :], acc.t[:, 0:48], pv.t[:, PV_BADA:PV_BADA + 48], ALU.add, [acc.b, pv.b], [adaT.b])
        STT(A12.t[:, 0:8], adaT.t[:, 8:16], 1.0, pv.t[:, PV_G1:PV_G1 + 8], ALU.add, ALU.mult, [adaT.b, pv.b], [A12.b])
        STT(A12.t[:, 8:16], adaT.t[:, 32:40], 1.0, pv.t[:, PV_G2:PV_G2 + 8], ALU.add, ALU.mult, [adaT.b, pv.b], [A12.b])
    S.barrier()
    if stop == "A":
        dump([(adaT.t[:, :], [adaT.b], 48, True), (A12.t[:, :], [A12.b], 16, True)])
        return
    SH1 = lambda c: adaT.t[:, 0 + c:1 + c]
    G1 = lambda c: adaT.t[:, 16 + c:17 + c]
    SH2 = lambda c: adaT.t[:, 24 + c:25 + c]
    G2 = lambda c: adaT.t[:, 40 + c:41 + c]
    A1 = lambda c: A12.t[:, c:c + 1]
    A2 = lambda c: A12.t[:, 8 + c:9 + c]
    cb = [adaT.b, A12.b, pv.b]

    def stats_rstd(es_tiles, src_chunks, T, rstd):
        sq = es_tiles["sq"]
        ssb = psb[SSB]
        pend = None
        for c in range(8):
            ap, bufs = src_chunks[c]
            sl = c % 2
            ACTV(sq.t[:, sl, :T], ap, AF.Square, bufs, [sq.bs[sl]])
            if pend is not None:
                pend()
            pend = (lambda c=c, sl=sl: MM(ssb.t[:, :T], onesf.t[:, 0:128], sq.t[:, sl, :T], c == 0, c == 7,
                                          [onesf.b, sq.bs[sl]], [ssb.b]))
        pend()
        ACTV(rstd.t[:, :T], ssb.t[:, :T], AF.Ln, [ssb.b, pv.b], [rstd.b], scale=1.0 / D, bias=pvc(PV_EPS))
        ACTV(rstd.t[:, :T], rstd.t[:, :T], AF.Exp, [rstd.b], [rstd.b], scale=-0.5)

    def load_T(tl, src_tiles, T, keep_xT):
        xt, xT = tl["xt"], tl["xT"]
        ntt = T // 128
        for tt in range(ntt):
            S.dma(xt.t[:, tt, :], src_tiles[tt], writes=[xt.bs[tt]])
        for c in range(8):
            bk = gbank()
            for tt in range(ntt):
                TR(bk.t[:, tt * 128:(tt + 1) * 128], xt.t[:, tt, c * 128:(c + 1) * 128], identf.t[:, :],
                   [xt.bs[tt], identf.b], [bk.b])
            CP("dve", xT.t[:, c, :T], bk.t[:, :T], [bk.b], [xT.bs[c]])
        return [(xT.t[:, c, :T], [xT.bs[c]]) for c in range(8)]

    def modulate(tl, T, rstd, Af, SHf, hT):
        xT, tmp = tl["xT"], tl["tmp"]
        for c in range(8):
            sl = c % 2
            STT(tmp.t[:, sl, :T], xT.t[:, c, :T], Af(c), rstd.t[:, :T], ALU.mult, ALU.mult,
                [xT.bs[c], rstd.b] + cb, [tmp.bs[sl]])
            ACTV(hT.t[:, c, :T], tmp.t[:, sl, :T], AF.Identity, [tmp.bs[sl]] + cb, [hT.bs[c]],
                 bias=SHf(c), scale=1.0)

    def load_w(dst_ap, src_ap, w):
        return S.dma(dst_ap, src_ap.rearrange("(c p) n -> p c n", p=128), writes=w, q="pool")

    def projT(w_ap_fn, wbuf, hT, T, M):
        bk = gbank()
        for c in range(8):
            MM(bk.t[:M, :T], w_ap_fn(c), hT.t[:, c, :T], c == 0, c == 7, [hT.bs[c], wbuf], [bk.b])
        return bk

    def par_sel(blk):
        return (PV_PAR0, PV_PAR1) if blk % 2 == 0 else (PV_PAR1, PV_PAR0)

    def attn_finish(Y, yT, sigma):
        psT = psTs[sigma % 2]
        for fc in range(4):
            TR(psT.t[:, fc * 128:(fc + 1) * 128], Y.t[:, fc * 128:(fc + 1) * 128],
               identb.t[:, :], [Y.b, identb.b], [psT.b])
        CP("act", yT.t[:, :, sigma * 128:(sigma + 1) * 128],
           psT.t[:, 0:512].rearrange("p (f q) -> p f q", f=4), [psT.b], [yT.b])

    def pv_norm(Ob, Y, h, rec):
        S.op("dve", lambda e: e.reciprocal(out=rec.t[:, 0:1], in_=Ob.t[:, 64:65]), [Ob.b], [rec.b])
        TS("dve", Y.t[:, h * 64:(h + 1) * 64], Ob.t[:, 0:64], rec.t[:, 0:1], None, ALU.mult, None,
           [Ob.b, rec.b], [Y.b])

    def pv_norm_act(Ob, Y, h, rec):
        ACTV(rec.t[:, 0:1], Ob.t[:, 64:65], AF.Ln, [Ob.b], [rec.b])
        ACTV(rec.t[:, 0:1], rec.t[:, 0:1], AF.Exp, [rec.b], [rec.b], scale=-1.0)
        ACTV(Y.t[:, h * 64:(h + 1) * 64], Ob.t[:, 0:64], AF.Identity, [Ob.b, rec.b], [Y.b], scale=rec.t[:, 0:1])

    def run_attention(ntile, front, back, norm, finish, norm_delay, look=1):
        jobs = [(h, jb) for h in range(8) for jb in range(0, ntile, 4)]
        ctx = {}
        for i in range(min(look, len(jobs))):
            ctx[i] = front(*jobs[i])
        pending = []
        for i, (h, jb) in enumerate(jobs):
            if i + look < len(jobs):
                ctx[i + look] = front(*jobs[i + look])
            back(h, jb, ctx.pop(i))
            if jb + 4 >= ntile:
                pending.append((i + norm_delay, h))
            while pending and pending[0][0] <= i:
                norm(pending.pop(0)[1])
        for _, h in pending:
            norm(h)
        finish()

    with ExitStack() as es:
        KF = sb(es, "KF", [128, 4, S_LEN], BF16, 16)
        KA = sb(es, "KA", [128, S_LEN], BF16, 16)
        VF = sb(es, "VF", [128, NT, 8 * 65], BF16, 16)
        QF = sb(es, "QF", [128, 4, NSLOT * 128], BF16, 16)
        QA = [sb(es, "QA%d" % v, [128, NSLOT * 128], BF16, 16) for v in range(3)]
        maskFb = sb(es, "maskFb", [128, 4 * 128], BF16)
        S.dma(maskFb.t[:], maskFd[:, :], writes=[maskFb.b], q="pool")
        MSET("pool", VF.t[:, :, :].rearrange("p t (h e) -> p t h e", h=8)[:, :, :, 64:65], 1.0, VF.bs)
        with ExitStack() as eb:
            wF = sb(eb, "wF", [128, 8, 1536 + 128], BF16)
            load_w(wF.t[:, :, 0:1536], w_in[:, 0:1536], [wF.b])
            load_w(wF.t[:, :, 1536:1664], wfpad[:, :], [wF.b])
            for dst, src, step in ((wgb, w_in[:, GF0:GF0 + 2048], 512), (wbfb, wbfd, 512), (wbdb, wbdd, 512),
                                   (wob, w_out, 1024), (w1b, w_mlp1, 256), (w2b, w_mlp2, 1024)):
                for r0 in range(0, dst.shape[0], step):
                    S.dma(dst[r0:r0 + step, :], src[r0:r0 + step, :], writes=[convb], q="pool")
            tl = {"xt": sb(eb, "xt", [128, 2, D], F32, 2), "xT": sb(eb, "xT", [128, 8, 256], F32, 8),
                  "sq": sb(eb, "sq", [128, 2, 256], F32, 2), "tmp": sb(eb, "tmp", [128, 2, 256], F32, 2)}
            tl2 = dict(tl)
            tl2["xT"] = sb(eb, "xTb", [128, 8, 256], F32, 8)
            tls = [tl, tl2]
            rstd = sb(eb, "rstd", [128, 256], F32)
            hTs = [sb(eb, "hT%d" % i, [128, 8, 256], BF16, 8) for i in range(2)]
            sc = [sb(eb, "scn%d" % i, [128, 256], F32) for i in range(4)]
            lv = [sb(eb, "lv%d" % i, [128, 256], BF16) for i in range(3)]
            carry = sb(eb, "carry", [128, 1], F32)
            qtmp = sb(eb, "qtmp", [128, 128], F32)
            MSET("dve", carry.t[:, :], 0.0, [carry.b])
            T = 256
            def proj_block(blk, hT):
                sig = blk
                selA, selB = par_sel(blk)
                kb = blk // 1
                for pr in range(4):
                    bk = projT(lambda c, pr=pr: wF.t[:, c, 512 + pr * 128: 512 + (pr + 1) * 128], wF.b, hT, T, 128)
                    CP("act", KF.t[:, pr, blk * 256:(blk + 1) * 256], bk.t[:, :T], [bk.b, wF.b], [KF.bs[kb]])
                for pr in range(4):
                    bk = projT(lambda c, pr=pr: wF.t[:, c, pr * 128:(pr + 1) * 128], wF.b, hT, T, 128)
                    TS("dve", qtmp.t[:, :], bk.t[:, 0:128], pvc(selA), None, ALU.mult, None, [bk.b, pv.b, wF.b], [qtmp.b])
                    STT(QF.t[:, pr, sig * 128:(sig + 1) * 128], bk.t[:, 128:256], pvc(selB), qtmp.t[:, :],
                        ALU.mult, ALU.add, [bk.b, qtmp.b, pv.b], [QF.bs[sig]])
                for tt in range(2):
                    bk = gbank()
                    for c in range(8):
                        MM(bk.t[:, :512], hT.t[:, c, tt * 128:(tt + 1) * 128], wF.t[:, c, 1024:1536], c == 0, c == 7,
                           [hT.bs[c], wF.b], [bk.b])
                    tile_i = blk * 2 + tt
                    CP("act", VF.t[:, tile_i, :].rearrange("p (h e) -> p h e", h=8)[:, :, 0:64],
                       bk.t[:, :512].rearrange("p (h d) -> p h d", h=8), [bk.b], [VF.bs[kb]])
                bk = projT(lambda c: wF.t[:, c, 1536:1664], wF.b, hT, T, 128)
                ACTV(sc[0].t[:, :], bk.t[:, :T], AF.Sigmoid, [bk.b, pv.b, wF.b], [sc[0].b], bias=pvc(PV_BF), scale=1.0)
                ACTV(sc[1].t[:, :], sc[0].t[:, :], AF.Ln, [sc[0].b], [sc[1].b])
                S.op("dve", lambda e: e.tensor_tensor_scan(out=sc[2].t[:, :], data0=onesf.t[:, 0:256], data1=sc[1].t[:, :],
                                                            initial=carry.t[:, 0:1], op0=ALU.mult, op1=ALU.add),
                     [sc[1].b, onesf.b, carry.b], [sc[2].b])
                CP("dve", carry.t[:, 0:1], sc[2].t[:, 255:256], [sc[2].b], [carry.b])
                TS("dve", lv[0].t[:, :], sc[2].t[:, :], -8.0, None, ALU.mult, None, [sc[2].b], [lv[0].b])
                STT(sc[3].t[:, :], sc[2].t[:, :], -8.0, lv[0].t[:, :], ALU.mult, ALU.subtract, [sc[2].b, lv[0].b], [sc[3].b])
                CP("dve", lv[1].t[:, :], sc[3].t[:, :], [sc[3].b], [lv[1].b])
                TT("dve", lv[2].t[:, :], sc[3].t[:, :], lv[1].t[:, :], ALU.subtract, [sc[3].b, lv[1].b], [lv[2].b])
                TS("dve", sc[0].t[:, :], lv[0].t[:, :], pvc(PV_SK + 0), None, ALU.mult, None, [lv[0].b, pv.b], [sc[0].b])
                STT(sc[0].t[:, :], lv[1].t[:, :], pvc(PV_SK + 1), sc[0].t[:, :], ALU.mult, ALU.add, [lv[1].b, sc[0].b], [sc[0].b])
                STT(sc[0].t[:, :], lv[2].t[:, :], pvc(PV_SK + 2), sc[0].t[:, :], ALU.mult, ALU.add, [lv[2].b, sc[0].b], [sc[0].b])
                TS("dve", KA.t[:, blk * 256:(blk + 1) * 256], sc[0].t[:, :], pvc(PV_SK + 3), None, ALU.add, None,
                   [sc[0].b, pv.b], [KA.bs[kb]])
                for v in range(3):
                    q0 = PV_SQ + 4 * v
                    TS("dve", sc[1].t[:, :], lv[0].t[:, :], pvc(q0 + 0), None, ALU.mult, None, [lv[0].b, pv.b], [sc[1].b])
                    STT(sc[1].t[:, :], lv[1].t[:, :], pvc(q0 + 1), sc[1].t[:, :], ALU.mult, ALU.add, [lv[1].b, sc[1].b], [sc[1].b])
                    STT(sc[1].t[:, :], lv[2].t[:, :], pvc(q0 + 2), sc[1].t[:, :], ALU.mult, ALU.add, [lv[2].b, sc[1].b], [sc[1].b])
                    TS("dve", sc[1].t[:, :], sc[1].t[:, :], pvc(q0 + 3), None, ALU.add, None, [sc[1].b, pv.b], [sc[1].b])
                    TS("dve", qtmp.t[:, :], sc[1].t[:, 0:128], pvc(selA), None, ALU.mult, None, [sc[1].b, pv.b], [qtmp.b])
                    STT(QA[v].t[:, sig * 128:(sig + 1) * 128], sc[1].t[:, 128:256], pvc(selB), qtmp.t[:, :],
                        ALU.mult, ALU.add, [sc[1].b, qtmp.b, pv.b], [QA[v].bs[sig]])
            prev = None
            for blk in range(16):
                hT = hTs[blk % 2]
                tl = tls[blk % 2]
                src = load_T(tl, [xall[blk * 256 + tt * 128: blk * 256 + (tt + 1) * 128, :] for tt in range(2)], T, False)
                if prev is not None:
                    proj_block(*prev)
                stats_rstd(tl, src, T, rstd)
                modulate(tl, T, rstd, A1, SH1, hT)
                prev = (blk, hT)
            proj_block(*prev)
        S.barrier()
        if stop == "Fb":
            dump([(KF.t[:, 0, 0:128], KF.bs, 128, False), (KF.t[:, 3, 3968:4096], KF.bs, 128, False),
                  (KA.t[:, 1000:1128], KA.bs, 128, False), (QF.t[:, 1, 640:768], QF.bs, 128, False),
                  (QA[1].t[:, 640:768], QA[1].bs, 128, False), (VF.t[:, 17, 0:130], VF.bs, 130, False)])
            return
        with ExitStack() as ea:
            Pr = sb(ea, "Pr", [128, 4, 512], BF16, 4)
            Y = sb(ea, "Yf", [128, 512], BF16)
            rec = sb(ea, "recf", [128, 1], F32)
            stF = {"pi": 0}
            for sigma in range(NSLOT):
                e_par = sigma % 2
                ntile = 2 * sigma + 2
                qs = slice(sigma * 128, (sigma + 1) * 128)

                def front(h, jb, sigma=sigma, e_par=e_par, ntile=ntile, qs=qs):
                    pr, p0 = h // 2, (h % 2) * 64
                    vq, base = h % 3, 32 * (h // 3)
                    nj = min(4, ntile - jb)
                    bk = gbank()
                    for jj in range(nj):
                        j = jb + jj
                        ks = slice(j * 128, (j + 1) * 128)
                        cols = bk.t[:, jj * 128:(jj + 1) * 128]
                        hm = j >= 2 * sigma
                        MM(cols, KF.t[p0:p0 + 64, pr, ks], QF.t[p0:p0 + 64, pr, qs], True, False,
                           [KF.bs[j // 2], QF.bs[sigma]], [bk.b])
                        MM(cols, KA.t[base:base + 18, ks], QA[vq].t[base:base + 18, qs], False, not hm,
                           [KA.bs[j // 2], QA[vq].bs[sigma]], [bk.b])
                        if hm:
                            mi = e_par * 2 + (j - 2 * sigma)
                            MM(cols, identb.t[:, :], maskFb.t[:, mi * 128:(mi + 1) * 128], False, True,
                               [identb.b, maskFb.b], [bk.b])
                    sl = stF["pi"] % 4
                    stF["pi"] += 1
                    ACTV(Pr.t[:, sl, :nj * 128], bk.t[:, :nj * 128], AF.Exp, [bk.b], [Pr.bs[sl]], scale=0.125)
                    return sl

                def back(h, jb, sl, ntile=ntile):
                    Ob = psb[OB0 + (h % 2)]
                    nj = min(4, ntile - jb)
                    for jj in range(nj):
                        j = jb + jj
                        MM(Ob.t[:, 0:65], Pr.t[:, sl, jj * 128:(jj + 1) * 128], VF.t[:, j, h * 65:(h + 1) * 65],
                           j == 0, j == ntile - 1, [Pr.bs[sl], VF.bs[j // 2]], [Ob.b])

                run_attention(ntile, front, back,
                              lambda h: pv_norm(psb[OB0 + (h % 2)], Y, h, rec),
                              lambda sigma=sigma: attn_finish(Y, yfoxT, sigma), 0)

    S.barrier()
    if stop == "F":
        dump([(yfoxT.t[:, fc, sg_ * 128:(sg_ + 1) * 128], [yfoxT.b], 128, False) for sg_ in (0, 5) for fc in range(4)])
        return
    with ExitStack() as es:
        KD = sb(es, "KD", [128, 4, S_LEN], BF16, 16)
        VD = sb(es, "VD", [128, NT, 8 * 65], BF16, 16)
        KI = sb(es, "KI", [128, S_LEN], BF16, 16)
        QD = sb(es, "QD", [128, 4, NSLOT * 128], BF16, 16)
        QI = sb(es, "QI", [128, 2, NSLOT * 128], BF16, 16)
        WQ = sb(es, "WQ", [128, NSLOT * 4], F32, 16)
        MSET("pool", VD.t[:, :, :].rearrange("p t (h e) -> p t h e", h=8)[:, :, :, 64:65], 1.0, VD.bs)
        with ExitStack() as eb:
            NW = 1536 + 256 + 128 + 4
            wD = sb(eb, "wD", [128, 8, NW], BF16)
            load_w(wD.t[:, :, 0:1536], w_in[:, QD0:QD0 + 1536], [wD.b])
            load_w(wD.t[:, :, 1536:1792], w_in[:, QI0:QI0 + 256], [wD.b])
            load_w(wD.t[:, :, 1792:1920], wkidup[:, :], [wD.b])
            load_w(wD.t[:, :, 1920:1924], w_in[:, WI0:WI0 + 4], [wD.b])
            tl = {"xt": sb(eb, "xt", [128, 2, D], F32, 2), "xT": sb(eb, "xT", [128, 8, 256], F32, 8),
                  "sq": sb(eb, "sq", [128, 2, 256], F32, 2), "tmp": sb(eb, "tmp", [128, 2, 256], F32, 2)}
            tl2 = dict(tl)
            tl2["xT"] = sb(eb, "xTb", [128, 8, 256], F32, 8)
            tls = [tl, tl2]
            rstd = sb(eb, "rstd", [128, 256], F32)
            hTs = [sb(eb, "hT%d" % i, [128, 8, 256], BF16, 8) for i in range(2)]
            qtmp = sb(eb, "qtmp", [128, 128], F32)
            T = 256
            def proj_block(blk, hT):
                sig = blk
                selA, selB = par_sel(blk)
                for pr in range(4):
                    bk = projT(lambda c, pr=pr: wD.t[:, c, 512 + pr * 128: 512 + (pr + 1) * 128], wD.b, hT, T, 128)
                    CP("act", KD.t[:, pr, blk * 256:(blk + 1) * 256], bk.t[:, :T], [bk.b, wD.b], [KD.bs[blk]])
                bk = projT(lambda c: wD.t[:, c, 1792:1920], wD.b, hT, T, 128)
                CP("act", KI.t[:, blk * 256:(blk + 1) * 256], bk.t[:, :T], [bk.b, wD.b], [KI.bs[blk]])
                for pr in range(4):
                    bk = projT(lambda c, pr=pr: wD.t[:, c, pr * 128:(pr + 1) * 128], wD.b, hT, T, 128)
                    TS("dve", qtmp.t[:, :], bk.t[:, 0:128], pvc(selA), None, ALU.mult, None, [bk.b, pv.b, wD.b], [qtmp.b])
                    STT(QD.t[:, pr, sig * 128:(sig + 1) * 128], bk.t[:, 128:256], pvc(selB), qtmp.t[:, :],
                        ALU.mult, ALU.add, [bk.b, qtmp.b, pv.b], [QD.bs[sig]])
                for ch in range(2):
                    bk = projT(lambda c, ch=ch: wD.t[:, c, 1536 + ch * 128: 1536 + (ch + 1) * 128], wD.b, hT, T, 128)
                    TS("dve", qtmp.t[:, :], bk.t[:, 0:128], pvc(selA), None, ALU.mult, None, [bk.b, pv.b, wD.b], [qtmp.b])
                    STT(QI.t[:, ch, sig * 128:(sig + 1) * 128], bk.t[:, 128:256], pvc(selB), qtmp.t[:, :],
                        ALU.mult, ALU.add, [bk.b, qtmp.b, pv.b], [QI.bs[sig]])
                for tt in range(2):
                    bk = gbank()
                    for c in range(8):
                        MM(bk.t[:, :512], hT.t[:, c, tt * 128:(tt + 1) * 128], wD.t[:, c, 1024:1536], c == 0, c == 7,
                           [hT.bs[c], wD.b], [bk.b])
                    tile_i = blk * 2 + tt
                    CP("act", VD.t[:, tile_i, :].rearrange("p (h e) -> p h e", h=8)[:, :, 0:64],
                       bk.t[:, :512].rearrange("p (h d) -> p h d", h=8), [bk.b], [VD.bs[blk]])
                bk = gbank()
                for tt in range(2):
                    for c in range(8):
                        MM(bk.t[:, tt * 4:(tt + 1) * 4], hT.t[:, c, tt * 128:(tt + 1) * 128], wD.t[:, c, 1920:1924],
                           c == 0, c == 7, [hT.bs[c], wD.b], [bk.b])
                sA = PV_PAR0S if selA == PV_PAR0 else PV_PAR1S
                sB = PV_PAR0S if selB == PV_PAR0 else PV_PAR1S
                TS("dve", qtmp.t[:, 0:4], bk.t[:, 0:4], pvc(sA), None, ALU.mult, None, [bk.b, pv.b], [qtmp.b])
                STT(WQ.t[:, sig * 4:(sig + 1) * 4], bk.t[:, 4:8], pvc(sB), qtmp.t[:, 0:4], ALU.mult, ALU.add,
                    [bk.b, qtmp.b, pv.b], [WQ.bs[sig]])
            prev = None
            for blk in range(16):
                hT = hTs[blk % 2]
                tl = tls[blk % 2]
                src = load_T(tl, [xall[blk * 256 + tt * 128: blk * 256 + (tt + 1) * 128, :] for tt in range(2)], T, False)
                if prev is not None:
                    proj_block(*prev)
                stats_rstd(tl, src, T, rstd)
                modulate(tl, T, rstd, A1, SH1, hT)
                prev = (blk, hT)
            proj_block(*prev)
        S.barrier()
        if stop == "Db":
            dump([(KD.t[:, 2, 1024:1152], KD.bs, 128, False), (KI.t[:, 1024:1152], KI.bs, 128, False),
                  (QD.t[:, 1, 640:768], QD.bs, 128, False), (QI.t[:, 1, 640:768], QI.bs, 128, False),
                  (WQ.t[:, 0:64], WQ.bs, 64, True), (VD.t[:, 17, 0:130], VD.bs, 130, False)])
            return
        with ExitStack() as ea:
            isc = sb(ea, "isc", [128, S_LEN], F32)
            mq = sb(ea, "mq", [128, S_LEN], BF16)
            maskTs = [sb(ea, "maskT%d" % i, [128, NT, 128], BF16) for i in range(2)]
            B8 = sb(ea, "B8", [128, 8 * 6 * 128], BF16)
            maskIs = sb(ea, "maskIs", [128, 512], F32)
            rl = sb(ea, "rl", [128, 2, 512], F32, 2)
            Pr = sb(ea, "Prd", [128, 4, 512], BF16, 4)
            Pm = sb(ea, "Pmd", [128, 4, 512], BF16, 4)
            Y = sb(ea, "Yd", [128, 512], BF16)
            recs = [sb(ea, "recd%d" % i, [128, 1], F32) for i in range(2)]
            small = sb(ea, "small", [128, 8], F32)
            steps = sb(ea, "steps", [128, NBIS + 1], F32)
            p2 = sb(ea, "p2", [128, NBIS + 1], F32)
            S.dma(maskIs.t[:], maskId[:, :], writes=[maskIs.b])
            for i in range(NBIS + 1):
                MSET("dve", p2.t[:, i:i + 1], 2.0 ** (-i), [p2.b])
            with ExitStack() as et:
                bst = sb(et, "bst", [128, 768], F32)
                for h in range(8):
                    S.dma(bst.t[:], bmatd[:, h * 768:(h + 1) * 768], writes=[bst.b])
                    TS("dve", B8.t[:, h * 768:(h + 1) * 768], bst.t[:, :], pvc(PV_B31 + h), 8.0, ALU.subtract, ALU.mult,
                       [bst.b, pv.b], [B8.b])
            S.barrier()
            if stop == "D0":
                dump([(B8.t[:, 0:512], [B8.b], 512, False), (p2.t[:, :], [p2.b], NBIS, True)])
                return
            st = {"pi": 0, "ri": 0}
            small = [small, sb(ea, "small2", [128, 8], F32)]
            steps = [steps, sb(ea, "steps2", [128, NBIS + 1], F32)]

            def prep_steps(sigma):
                out = []
                e_par = sigma % 2
                ntile = 2 * sigma + 2
                nk = ntile * 128
                qs = slice(sigma * 128, (sigma + 1) * 128)
                mT = maskTs[sigma % 2]
                sm, stp = small[sigma % 2], steps[sigma % 2]
                R, lo, thr, cnt, dd = (sm.t[:, i:i + 1] for i in range(5))

                def idx(k0, wd, hi):
                    kbufs = [KI.bs[(k0 + t * 256) // 256] for t in range((wd + 255) // 256)]
                    p0, ch = (hi % 2) * 64, hi // 2
                    bk = gbank()
                    MM(bk.t[:, :wd], QI.t[p0:p0 + 64, ch, qs], KI.t[p0:p0 + 64, k0:k0 + wd], True, True,
                       [QI.bs[sigma]] + kbufs, [bk.b])
                    wcol = WQ.t[:, sigma * 4 + hi: sigma * 4 + hi + 1]
                    if hi == 0:
                        TS("dve", isc.t[:, k0:k0 + wd], bk.t[:, :wd], 0.0, wcol, ALU.max, ALU.mult,
                           [bk.b, WQ.bs[sigma]], [isc.b])
                    else:
                        sl = st["ri"] % 2
                        st["ri"] += 1
                        ACTV(rl.t[:, sl, :wd], bk.t[:, :wd], AF.Relu, [bk.b], [rl.bs[sl]])
                        STT(isc.t[:, k0:k0 + wd], rl.t[:, sl, :wd], wcol, isc.t[:, k0:k0 + wd], ALU.mult, ALU.add,
                            [rl.bs[sl], WQ.bs[sigma], isc.b], [isc.b])
                for k0 in range(0, nk, 512):
                    wd = min(512, nk - k0)
                    for hi in range(4):
                        out.append(lambda k0=k0, wd=wd, hi=hi: idx(k0, wd, hi))

                def init():
                    S.op("dve", lambda e: e.tensor_reduce(out=R, in_=isc.t[:, :nk], axis=AX.X, op=ALU.max,
                                                          apply_absolute_value=True), [isc.b], [sm.b])
                    TT("dve", isc.t[:, 2 * sigma * 128: nk], isc.t[:, 2 * sigma * 128: nk],
                       maskIs.t[:, e_par * 256:(e_par + 1) * 256], ALU.add, [isc.b, maskIs.b], [isc.b])
                    TS("dve", stp.t[:, :], p2.t[:, :], R, None, ALU.mult, None, [p2.b, sm.b], [stp.b])
                    TS("dve", thr, R, 0.0, None, ALU.mult, None, [sm.b], [sm.b])
                out.append(init)

                def bis(it):
                    TS("dve", mq.t[:, :nk], isc.t[:, :nk], thr, 0.0, ALU.is_ge, ALU.add, [isc.b, sm.b], [mq.b, sm.b],
                       accum_out=cnt)
                    STT(dd, cnt, 256.0, stp.t[:, it:it + 1], ALU.is_ge, ALU.mult, [sm.b, stp.b], [sm.b])
                    TS("dve", thr, thr, dd, stp.t[:, it + 1:it + 2], ALU.add, ALU.subtract, [sm.b, stp.b], [sm.b])
                for it in range(NBIS):
                    out.append(lambda it=it: bis(it))
                out.append(lambda: TS("dve", lo, thr, stp.t[:, NBIS:NBIS + 1], None, ALU.subtract, None, [sm.b, stp.b], [sm.b]))
                out.append(lambda: TS("dve", mq.t[:, :nk], isc.t[:, :nk], lo, None, ALU.is_ge, None, [isc.b, sm.b], [mq.b]))

                def tr(jb):
                    nj = min(4, ntile - jb)
                    psT = psTs[(jb // 4) % 2]
                    for jj in range(nj):
                        j = jb + jj
                        TR(psT.t[:, jj * 128:(jj + 1) * 128], mq.t[:, j * 128:(j + 1) * 128],
                           identb.t[:, :], [mq.b, identb.b], [psT.b])
                    CP("act", mT.t[:, jb:jb + nj, :],
                       psT.t[:, 0:nj * 128].rearrange("p (j q) -> p j q", j=nj), [psT.b], [mT.b])
                post = [(lambda jb=jb: tr(jb)) for jb in range(0, ntile, 4)]
                return out, post

            def attn_slot(sigma):
                e_par = sigma % 2
                ntile = 2 * sigma + 2
                qs = slice(sigma * 128, (sigma + 1) * 128)
                mT = maskTs[sigma % 2]

                def front(h, jb):
                    pr, p0 = h // 2, (h % 2) * 64
                    nj = min(4, ntile - jb)
                    bk = gbank()
                    for jj in range(nj):
                        j = jb + jj
                        ks = slice(j * 128, (j + 1) * 128)
                        cols = bk.t[:, jj * 128:(jj + 1) * 128]
                        wh = j - (2 * sigma - 1)
                        near = 0 <= wh <= 2
                        MM(cols, KD.t[p0:p0 + 64, pr, ks], QD.t[p0:p0 + 64, pr, qs], True, not near,
                           [KD.bs[j // 2], QD.bs[sigma]], [bk.b])
                        if near:
                            bi = (h * 6 + e_par * 3 + wh) * 128
                            MM(cols, identb.t[:, :], B8.t[:, bi:bi + 128], False, True, [identb.b, B8.b], [bk.b])
                    sl = st["pi"] % 4
                    st["pi"] += 1
                    ACTV(Pr.t[:, sl, :nj * 128], bk.t[:, :nj * 128], AF.Exp, [bk.b, pv.b], [Pr.bs[sl]],
                         scale=0.125, bias=pvc(PV_B31 + h))
                    TT("pool", Pm.t[:, sl, :nj * 128], Pr.t[:, sl, :nj * 128],
                       mT.t[:, jb:jb + nj, :].rearrange("p j q -> p (j q)"), ALU.mult,
                       [Pr.bs[sl], mT.b], [Pm.bs[sl]])
                    return sl

                def back(h, jb, sl):
                    Ob = psb[OB0 + (h % 2)]
                    nj = min(4, ntile - jb)
                    for jj in range(nj):
                        j = jb + jj
                        MM(Ob.t[:, 0:65], Pm.t[:, sl, jj * 128:(jj + 1) * 128], VD.t[:, j, h * 65:(h + 1) * 65],
                           j == 0, j == ntile - 1, [Pm.bs[sl], VD.bs[j // 2]], [Ob.b])

                run_attention(ntile, front, back,
                              lambda h: pv_norm_act(psb[OB0 + (h % 2)], Y, h, recs[h % 2]),
                              lambda: attn_finish(Y, ydsaT, sigma), 1, look=2)

            pre, post = prep_steps(0)
            for fn in pre + post:
                fn()
            for sigma in range(_DBG_DSLOTS):
                pre, post = prep_steps(sigma + 1) if sigma + 1 < _DBG_DSLOTS else ([], [])
                for fn in pre:
                    fn()
                attn_slot(sigma)
                for fn in post:
                    fn()
    S.barrier()
    if stop == "D":
        dump([(ydsaT.t[:, fc, sg_ * 128:(sg_ + 1) * 128], [ydsaT.b], 128, False) for sg_ in (0, 5) for fc in range(4)])
        return
    with ExitStack() as es:
        wbf = sb(es, "wbf", [128, 4, D], BF16)
        wbd = sb(es, "wbd", [128, 4, D], BF16)
        wo = sb(es, "wo", [128, 8, D], BF16)
        S.dma(wbf.t[:], wbfb.rearrange("(c p) n -> p c n", p=128), reads=[convb], writes=[wbf.b])
        S.dma(wbd.t[:], wbdb.rearrange("(c p) n -> p c n", p=128), reads=[convb], writes=[wbd.b])
        S.dma(wo.t[:, :, :], wob.rearrange("(c p) n -> p c n", p=128), reads=[convb], writes=[wo.b])
        tl = {"xt": sb(es, "xt", [128, 4, D], F32, 4), "xT": sb(es, "xT", [128, 8, 512], F32, 8),
              "sq": sb(es, "sq", [128, 2, 512], F32, 2), "tmp": sb(es, "tmp", [128, 2, 512], F32, 2)}
        xT = tl["xT"]
        rstd = sb(es, "rstd", [128, 512], F32)
        hT = sb(es, "hT", [128, 8, 512], BF16, 8)
        mg = sb(es, "mg", [128, 8, 512], BF16, 8)
        aT = sb(es, "aT", [128, 32, 512], BF16, 32)
        wg = sb(es, "wg", [128, 2, 8, 256], BF16, 2)
        sg = sb(es, "sg", [128, 2, 512], F32, 2)
        tg = sb(es, "tg", [128, 2, 512], F32, 2)
        w1t = sb(es, "w1t", [128, 2, 8, 512], BF16, 2)
        w2t = sb(es, "w2t", [128, 4, 512], BF16, 4)
        rr = sb(es, "rr", [128, 2, 512], F32, 2)
        ot = sb(es, "ot", [128, 2, D], F32, 2)
        T = 512
        w2i = 0
        last_out = []
        for g in range(4):
            gs = slice(g * 512, (g + 1) * 512)
            src = load_T(tl, [xown[g * 512 + tt * 128: g * 512 + (tt + 1) * 128, :] for tt in range(4)], T, True)
            stats_rstd(tl, src, T, rstd)
            modulate(tl, T, rstd, A1, SH1, hT)
            for oc in range(8):
                sl = oc % 2
                S.dma(wg.t[:, sl, :, 0:128], wgb[:, oc * 128:(oc + 1) * 128].rearrange("(c p) n -> p c n", p=128),
                      reads=[convb], writes=[wg.bs[sl]])
                S.dma(wg.t[:, sl, :, 128:256], wgb[:, 1024 + oc * 128: 1024 + (oc + 1) * 128].rearrange("(c p) n -> p c n", p=128),
                      reads=[convb], writes=[wg.bs[sl]])
                for br, (wb_, yT_) in enumerate(((wbf, yfoxT), (wbd, ydsaT))):
                    bk = gbank()
                    for c in range(8):
                        MM(bk.t[:, :T], wg.t[:, sl, c, br * 128:(br + 1) * 128], hT.t[:, c, :T], c == 0, c == 7,
                           [wg.bs[sl], hT.bs[c]], [bk.b])
                    ACTV(sg.t[:, br, :], bk.t[:, :T], AF.Sigmoid, [bk.b], [sg.bs[br]])
                    bk2 = gbank()
                    for fc in range(4):
                        MM(bk2.t[:, :T], wb_.t[:, fc, oc * 128:(oc + 1) * 128], yT_.t[:, fc, gs], fc == 0, fc == 3,
                           [wb_.b, yT_.b], [bk2.b])
                    TT("dve", tg.t[:, br, :], sg.t[:, br, :], bk2.t[:, :T], ALU.mult, [sg.bs[br], bk2.b], [tg.bs[br]])
                TT("pool", mg.t[:, oc, :], tg.t[:, 0, :], tg.t[:, 1, :], ALU.add, [tg.bs[0], tg.bs[1]], [mg.bs[oc]])
            for oc in range(8):
                bk = gbank()
                for c in range(8):
                    MM(bk.t[:, :T], wo.t[:, c, oc * 128:(oc + 1) * 128], mg.t[:, c, :], c == 0, c == 7,
                       [wo.b, mg.bs[c]], [bk.b])
                STT(xT.t[:, oc, :], bk.t[:, :T], G1(oc), xT.t[:, oc, :], ALU.mult, ALU.add, [bk.b, xT.bs[oc]] + cb, [xT.bs[oc]])
            stats_rstd(tl, [(xT.t[:, c, :], [xT.bs[c]]) for c in range(8)], T, rstd)
            modulate(tl, T, rstd, A2, SH2, hT)
            for fb in range(8):
                sl = fb % 2
                S.dma(w1t.t[:, sl, :, :], w1b[:, fb * 512:(fb + 1) * 512].rearrange("(c p) n -> p c n", p=128),
                      reads=[convb], writes=[w1t.bs[sl]])
                for f4 in range(4):
                    fcn = fb * 4 + f4
                    bk = gbank()
                    for c in range(8):
                        MM(bk.t[:, :T], w1t.t[:, sl, c, f4 * 128:(f4 + 1) * 128], hT.t[:, c, :], c == 0, c == 7,
                           [w1t.bs[sl], hT.bs[c]], [bk.b])
                    rs = fcn % 2
                    ACTV(rr.t[:, rs, :], bk.t[:, :T], AF.Relu, [bk.b], [rr.bs[rs]])
                    TT("pool", aT.t[:, fcn, :], rr.t[:, rs, :], rr.t[:, rs, :], ALU.mult, [rr.bs[rs]], [aT.bs[fcn]])
            for half in range(2):
                accs = [psb[2], psb[3], psb[4], psb[5]]
                for fcn in range(32):
                    sl = w2i % 4
                    w2i += 1
                    S.dma(w2t.t[:, sl, :], w2b[fcn * 128:(fcn + 1) * 128, half * 512:(half + 1) * 512],
                          reads=[convb], writes=[w2t.bs[sl]])
                    for o4 in range(4):
                        MM(accs[o4].t[:, :T], w2t.t[:, sl, o4 * 128:(o4 + 1) * 128], aT.t[:, fcn, :], fcn == 0, fcn == 31,
                           [w2t.bs[sl], aT.bs[fcn]], [accs[o4].b])
                for o4 in range(4):
                    oc = half * 4 + o4
                    STT(xT.t[:, oc, :], accs[o4].t[:, :T], G2(oc), xT.t[:, oc, :], ALU.mult, ALU.add,
                        [accs[o4].b, xT.bs[oc]] + cb, [xT.bs[oc]])
            stats_rstd(tl, [(xT.t[:, c, :], [xT.bs[c]]) for c in range(8)], T, rstd)
            for c in range(8):
                STT(xT.t[:, c, :], xT.t[:, c, :], pv.t[:, PV_GF + c:PV_GF + c + 1], rstd.t[:, :T], ALU.mult, ALU.mult,
                    [xT.bs[c], rstd.b, pv.b], [xT.bs[c]])
            for tt in range(4):
                osl = tt % 2
                for cbk in range(2):
                    bk = gbank()
                    for c4 in range(4):
                        c = cbk * 4 + c4
                        TR(bk.t[:, c4 * 128:(c4 + 1) * 128], xT.t[:, c, tt * 128:(tt + 1) * 128], identf.t[:, :],
                           [xT.bs[c], identf.b], [bk.b])
                    CP("act" if cbk == 0 else "dve", ot.t[:, osl, cbk * 512:(cbk + 1) * 512], bk.t[:, :512], [bk.b], [ot.bs[osl]])
                t = S.dma(outd[g * 512 + tt * 128: g * 512 + (tt + 1) * 128, :], ot.t[:, osl, :], reads=[ot.bs[osl]])
                last_out.append(t)
        S.wait("sp", last_out)
    ES.close()
    return nc


def _t5_bucket(dist):
    n = np.maximum(dist, 0)
    max_exact = 16
    nf = np.maximum(n, 1).astype(np.float32)
    large = max_exact + (np.log(nf / np.float32(max_exact)) / np.float32(math.log(128 / max_exact))
                         * np.float32(32 - max_exact)).astype(np.int32)
    large = np.minimum(large, 31)
    return np.where(n < max_exact, n, large)


def _own_tiles(p):
    tiles = []
    for m in range(8):
        tiles.append(4 * m + p)
        tiles.append(4 * m + 3 - p)
    return tiles


_NC_CACHE = {}


def kernel(x, c, w_ada, b_ada, g_norm1, w_in, b_forget, rel_bias, w_branch_fox, w_branch_dsa, w_out,
           g_norm2, w_mlp1, w_mlp2, g_final):
    f32 = np.float32
    x = np.asarray(x, f32)
    c = np.asarray(c, f32)
    w_in = np.ascontiguousarray(np.asarray(w_in, f32))
    b_ada = np.asarray(b_ada, f32)
    rel_bias = np.asarray(rel_bias, f32)
    b_forget = np.asarray(b_forget, f32)
    col = lambda v: np.ascontiguousarray(np.asarray(v, f32).reshape(-1, 128).T)

    wfpad = np.zeros((D, 128), f32)
    bfcol = np.zeros((128, 1), f32)
    for h in range(8):
        base = 32 * (h // 3) + 6 * (h % 3)
        for r in range(6):
            wfpad[:, base + r] = w_in[:, FL0 + h]
            bfcol[base + r, 0] = b_forget[h]
    wkidup = np.ascontiguousarray(np.concatenate([w_in[:, KI0:KI0 + 64], w_in[:, KI0:KI0 + 64]], axis=1))
    ident = np.eye(128, dtype=f32)
    rows = np.arange(128)
    rmod = rows % 32
    rv, rr = rmod // 6, rmod % 6
    act = rmod < 18
    selK = np.stack([act & (rr == 3), act & (rr == 4), act & (rr == 5), act & (rr < 3)], 1).astype(f32)
    selQ = np.zeros((128, 12), f32)
    for v in range(3):
        mine = act & (rv == v)
        selQ[:, 4 * v + 0] = -(mine & (rr == 0)).astype(f32)
        selQ[:, 4 * v + 1] = -(mine & (rr == 1)).astype(f32)
        selQ[:, 4 * v + 2] = -(mine & (rr == 2)).astype(f32)
        selQ[:, 4 * v + 3] = (mine & (rr >= 3)).astype(f32)

    in_maps = []
    own = []
    for core in range(8):
        b, p = core // 2, core % 2
        tiles = _own_tiles(p)
        own.append(tiles)
        xb = x[b]
        xo = np.ascontiguousarray(xb.reshape(NT, 128, D)[tiles].reshape(NSLOT * 128, D))
        pvv = np.zeros((128, NPV), f32)
        pvv[:, PV_C:PV_C + 8] = col(c[b])
        pvv[:, PV_BADA:PV_BADA + 48] = col(b_ada)
        pvv[:, PV_G1:PV_G1 + 8] = col(g_norm1)
        pvv[:, PV_G2:PV_G2 + 8] = col(g_norm2)
        pvv[:, PV_GF:PV_GF + 8] = col(g_final)
        pvv[:, PV_PAR0] = 1.0 if p == 0 else 0.0
        pvv[:, PV_PAR1] = 1.0 if p == 1 else 0.0
        pvv[:, PV_PAR0S] = 0.0625 if p == 0 else 0.0
        pvv[:, PV_PAR1S] = 0.0625 if p == 1 else 0.0
        pvv[:, PV_BF:PV_BF + 1] = bfcol
        pvv[:, PV_SK:PV_SK + 4] = selK
        pvv[:, PV_SQ:PV_SQ + 12] = selQ
        pvv[:, PV_B31:PV_B31 + 8] = rel_bias[31][None, :]
        pvv[:, PV_EPS] = 1e-6
        kk = rows[:, None]
        qq = rows[None, :]
        maskF = np.zeros((128, 4, 128), f32)
        maskI = np.zeros((128, 2, 256), f32)
        bmat = np.zeros((128, 8, 2, 3, 128), f32)
        for e in range(2):
            qt = p if e == 0 else 3 - p
            for which in range(2):
                kt = 2 * e + which
                ok = (kt * 128 + kk) <= (qt * 128 + qq)
                maskF[:, e * 2 + which, :] = np.where(ok, 0.0, NEG)
                maskI[:, e, which * 128:(which + 1) * 128] = np.where(ok.T, 0.0, -1e30)
            for w3 in range(3):
                kt = 2 * e - 1 + w3
                dist = (qt * 128 + qq) - (kt * 128 + kk)
                bk = _t5_bucket(dist)
                bmat[:, :, e, w3, :] = np.transpose(rel_bias[bk], (0, 2, 1))
        in_maps.append({
            "xall": np.ascontiguousarray(xb), "xown": xo, "pv": pvv,
            "w_ada": np.asarray(w_ada, f32), "w_in": w_in, "wfpad": wfpad, "wkidup": wkidup, "ident": ident,
            "maskF": np.ascontiguousarray(maskF.reshape(128, 512)),
            "maskI": np.ascontiguousarray(maskI.reshape(128, 512)),
            "bmat": np.ascontiguousarray(bmat.reshape(128, 8 * 6 * 128)),
            "w_branch_fox": np.asarray(w_branch_fox, f32), "w_branch_dsa": np.asarray(w_branch_dsa, f32),
            "w_out": np.asarray(w_out, f32), "w_mlp1": np.asarray(w_mlp1, f32), "w_mlp2": np.asarray(w_mlp2, f32),
        })
    if "nc" not in _NC_CACHE:
        _NC_CACHE["nc"] = build_program()
    nc = _NC_CACHE["nc"]
    res = run_bass_kernel_spmd(nc, in_maps, core_ids=list(range(8)))
    out = np.zeros((4, S_LEN, D), f32)
    for core in range(8):
        b = core // 2
        o = np.asarray(res.results[core]["out"], f32).reshape(NSLOT, 128, D)
        out[b].reshape(NT, 128, D)[own[core]] = o
    return out
```
